# Optimizing a Trainium2 kernel written in Bass

```python
import jax, jax.numpy as jnp
from jax import lax
import numpy as np

D_MODEL = 1024
BATCH = 16
SEQ = 2048
DEPTH = 1

PLE_DIM = 256
CONV_DIM = 512
CONV_WIDTH = 31
POOL_DIM = 512
POOL_WINDOWS = (2, 4, 8, 16)
POOL_GROUPS = len(POOL_WINDOWS)
POOL_GROUP_DIM = POOL_DIM // POOL_GROUPS
POOL_OUT_GROUP_DIM = D_MODEL // POOL_GROUPS
N_BRANCHES = 2
IN_COLS = 2 * CONV_DIM + POOL_DIM + N_BRANCHES * D_MODEL
N_EXPERTS = 16
EXPERT_FF = 1024
CAPACITY_FACTOR = 2
EPS = 1e-6

kernel_name = "hybrid_conv_pool_ec_moe_encoder_block"


def rms_norm(x, g):
    xf = x.astype(jnp.float32)
    y = xf * lax.rsqrt(jnp.mean(xf * xf, axis=-1, keepdims=True) + EPS)
    return (y * g.astype(jnp.float32)).astype(x.dtype)


def layer_norm(x, g, b):
    xf = x.astype(jnp.float32)
    mu = jnp.mean(xf, axis=-1, keepdims=True)
    xc = xf - mu
    var = jnp.mean(xc * xc, axis=-1, keepdims=True)
    y = xc * lax.rsqrt(var + EPS) * g.astype(jnp.float32) + b.astype(jnp.float32)
    return y.astype(x.dtype)


def conformer_conv(a, gate, conv_w, conv_b, ln_g, ln_b, w_out):
    v = a * jax.nn.sigmoid(gate)
    half = CONV_WIDTH // 2
    v = lax.conv_general_dilated(
        v, conv_w[:, None, :].astype(v.dtype), window_strides=(1,),
        padding=[(half, half)], dimension_numbers=("NWC", "WIO", "NWC"),
        feature_group_count=CONV_DIM) + conv_b
    v = jax.nn.silu(layer_norm(v, ln_g, ln_b))
    return jnp.einsum("bsc,cd->bsd", v, w_out)


def multiscale_pool(u, w_pool, scale):
    b, s, _ = u.shape
    ug = u.astype(jnp.float32).reshape(b, s, POOL_GROUPS, POOL_GROUP_DIM)
    c = jnp.concatenate(
        [jnp.zeros((b, 1, POOL_GROUPS, POOL_GROUP_DIM), jnp.float32),
         jnp.cumsum(ug, axis=1)], axis=1)
    t = jnp.arange(s)
    outs = []
    for g, w in enumerate(POOL_WINDOWS):
        lo = jnp.clip(t - w // 2, 0, s - 1)
        hi = jnp.clip(t + (w - w // 2) - 1, 0, s - 1)
        cnt = (hi - lo + 1).astype(jnp.float32)[None, :, None]
        mean = (c[:, hi + 1, g] - c[:, lo, g]) / cnt
        outs.append(mean - ug[:, :, g])
    d = jnp.stack(outs, axis=2).astype(u.dtype)
    y = jnp.einsum("bsgc,gce->bsge", d, w_pool).reshape(b, s, D_MODEL)
    return y * scale


def expert_choice_ffn(h, w_router, w_gate, w_up, w_down):
    b, s, d = h.shape
    cap = max(1, CAPACITY_FACTOR * s // N_EXPERTS)
    aff = jax.nn.softmax(jnp.einsum("bsd,de->bse", h, w_router).astype(jnp.float32), axis=-1)
    top_val, top_idx = lax.top_k(jnp.swapaxes(aff, 1, 2), cap)
    xg = jax.vmap(lambda hb, ib: hb[ib])(h, top_idx)
    hid = jax.nn.silu(jnp.einsum("becd,edf->becf", xg, w_gate)) * \
        jnp.einsum("becd,edf->becf", xg, w_up)
    ye = jnp.einsum("becf,efd->becd", hid, w_down) * top_val[..., None].astype(h.dtype)
    return jax.vmap(
        lambda yb, ib: jnp.zeros((s, d), yb.dtype).at[ib.reshape(-1)].add(yb.reshape(-1, d))
    )(ye, top_idx)


def setup_inputs(seed: int = 0) -> dict:
    key = jax.random.key(seed)
    ks = jax.random.split(key, 24)
    f32 = jnp.float32
    nrm = lambda k, shape, scale: jax.random.normal(k, shape, f32) * scale
    gain = lambda k, shape: 1.0 + 0.02 * jax.random.normal(k, shape, f32)
    L = DEPTH
    return {
        "x": jax.random.normal(ks[0], (BATCH, SEQ, D_MODEL), f32),
        "p": jax.random.normal(ks[1], (DEPTH, BATCH, SEQ, PLE_DIM), f32),
        "norm1_g": gain(ks[2], (L, D_MODEL)),
        "w_in": nrm(ks[3], (L, D_MODEL, IN_COLS), D_MODEL ** -0.5),
        "b_gate": nrm(ks[4], (L, N_BRANCHES * D_MODEL), 0.02),
        "conv_w": nrm(ks[5], (L, CONV_WIDTH, CONV_DIM), CONV_WIDTH ** -0.5),
        "conv_b": nrm(ks[6], (L, CONV_DIM), 0.02),
        "conv_ln_g": gain(ks[7], (L, CONV_DIM)),
        "conv_ln_b": nrm(ks[8], (L, CONV_DIM), 0.02),
        "w_conv_out": nrm(ks[9], (L, CONV_DIM, D_MODEL), CONV_DIM ** -0.5),
        "w_pool": nrm(ks[10], (L, POOL_GROUPS, POOL_GROUP_DIM, POOL_OUT_GROUP_DIM), POOL_GROUP_DIM ** -0.5),
        "pool_scale": gain(ks[11], (L, D_MODEL)),
        "w_out": nrm(ks[12], (L, D_MODEL, D_MODEL), D_MODEL ** -0.5),
        "norm2_g": gain(ks[13], (L, D_MODEL)),
        "w_router": nrm(ks[14], (L, D_MODEL, N_EXPERTS), D_MODEL ** -0.5),
        "w_exp_gate": nrm(ks[15], (L, N_EXPERTS, D_MODEL, EXPERT_FF), D_MODEL ** -0.5),
        "w_exp_up": nrm(ks[16], (L, N_EXPERTS, D_MODEL, EXPERT_FF), D_MODEL ** -0.5),
        "w_exp_down": nrm(ks[17], (L, N_EXPERTS, EXPERT_FF, D_MODEL), EXPERT_FF ** -0.5),
        "norm3_g": gain(ks[18], (L, D_MODEL)),
        "w_ple_gate": nrm(ks[19], (L, D_MODEL, D_MODEL), D_MODEL ** -0.5),
        "b_ple_gate": nrm(ks[20], (L, D_MODEL), 0.02),
        "w_ple": nrm(ks[21], (L, PLE_DIM, D_MODEL), PLE_DIM ** -0.5),
        "ple_norm_g": gain(ks[22], (L, D_MODEL)),
        "final_g": gain(ks[23], (D_MODEL,)),
    }


def reference(x, p, norm1_g, w_in, b_gate, conv_w, conv_b, conv_ln_g, conv_ln_b,
              w_conv_out, w_pool, pool_scale, w_out, norm2_g, w_router,
              w_exp_gate, w_exp_up, w_exp_down, norm3_g, w_ple_gate, b_ple_gate,
              w_ple, ple_norm_g, final_g):
    c1 = CONV_DIM
    c2 = 2 * CONV_DIM
    c3 = c2 + POOL_DIM
    c4 = c3 + D_MODEL
    for i in range(DEPTH):
        h = rms_norm(x, norm1_g[i])
        z = jnp.einsum("bsd,dk->bsk", h, w_in[i])
        gates = jax.nn.sigmoid(z[..., c3:] + b_gate[i])
        y_conv = conformer_conv(z[..., :c1], z[..., c1:c2], conv_w[i], conv_b[i],
                                conv_ln_g[i], conv_ln_b[i], w_conv_out[i])
        y_pool = multiscale_pool(z[..., c2:c3], w_pool[i], pool_scale[i])
        merged = gates[..., :D_MODEL] * y_conv + gates[..., D_MODEL:] * y_pool
        x = x + jnp.einsum("bsd,de->bse", merged, w_out[i])
        x = x + expert_choice_ffn(rms_norm(x, norm2_g[i]), w_router[i],
                                  w_exp_gate[i], w_exp_up[i], w_exp_down[i])
        g = jax.nn.sigmoid(jnp.einsum("bsd,de->bse", rms_norm(x, norm3_g[i]), w_ple_gate[i]) + b_ple_gate[i])
        e = rms_norm(jnp.einsum("bsq,qd->bsd", p[i], w_ple[i]), ple_norm_g[i])
        x = x + g * e
    return rms_norm(x, final_g)
```

```python
import numpy as np
from contextlib import ExitStack
import concourse.bass as bass
import concourse.mybir as mybir
from concourse.bass_utils import run_bass_kernel_spmd

F32 = mybir.dt.float32
BF16 = mybir.dt.bfloat16
I32 = mybir.dt.int32
U32 = mybir.dt.uint32
AF = mybir.ActivationFunctionType
ALU = mybir.AluOpType
AX = mybir.AxisListType

S = 2048
D = 1024
NSEQ = 2
TOK = NSEQ * S
NE = 16
CAP = 256
EPS = 1e-6
K = 1024
DTSIZE = {F32: 4, BF16: 2, I32: 4, U32: 4}
COMPUTE = ('pe', 'act', 'dve', 'pool')


class CostProbe:
    def __init__(self, eng):
        self.eng = eng
        self.cost = 0.0
        self.lat = 0.0
        self.cls = None

    def __getattr__(self, name):
        def f(*args, **kw):
            out = kw.get('out', args[0] if args else None)
            shape = tuple(out.shape)
            free = 1
            for d in shape[1:]:
                free *= d
            if name == 'matmul':
                lhsT = kw.get('lhsT')
                mult = 4.0 if (lhsT is not None and lhsT.dtype == F32) else 1.0
                self.cost += (0.03 + free / 1950.0) * mult
            elif name == 'transpose':
                self.cost += 0.07
            elif name in ('dma_start', 'indirect_dma_start'):
                nbytes = free * shape[0] * DTSIZE.get(out.dtype, 4)
                self.cost += 0.1 if self.eng == 'sp' else 1.0
                self.lat = max(self.lat, 2.0 + nbytes / 150e3)
            else:
                per = 0.0023 if self.eng == 'pool' else 0.00105
                self.cost += 0.2 + free * per
                if name == 'activation':
                    fn = kw.get('func', args[2] if len(args) > 2 else None)
                    if fn in (AF.Sigmoid, AF.Silu):
                        self.cls = 'sig'
                    elif fn == AF.Sqrt:
                        self.cls = 'sqrt'
                    elif fn in (AF.Exp, AF.Ln):
                        self.cls = 'exp'
            return self
        return f


class Op:
    __slots__ = ('idx', 'eng', 'emit', 'dma', 'ndma', 'deps', 'cost', 'lat', 'rec', 'seg', 'cls')


class Prog:
    ENG = ['pe', 'act', 'dve', 'pool', 'sp']
    WINDOW = 48
    LAT = 0.2
    TBL = 1.3
    cur_cls = None
    PERSIST = ('w16',)
    BACKGROUND = ('cv',)

    def __init__(self, nc, stack):
        self.nc = nc
        self.stack = stack
        self.ops = []
        self.segs = [[]]
        self.res = {}
        self.nbank = 0
        self.ncol = 0
        self.sems = {}
        for e in COMPUTE:
            self._sem(e)

    def _sem(self, name):
        if name not in self.sems:
            self.sems[name] = self.stack.enter_context(self.nc.semaphore("s_" + name))
        return self.sems[name]

    def bank(self):
        b = self.nbank % 8
        self.nbank += 1
        return b

    def col(self):
        c = self.ncol % 256
        self.ncol += 1
        return c

    def op(self, eng, emit, reads=(), writes=(), dma=None, ndma=1, after=()):
        o = Op()
        o.idx = len(self.ops)
        o.eng, o.emit, o.dma, o.ndma = eng, emit, dma, ndma
        deps = set()
        for r in reads:
            st = self.res.get(r)
            if st and st[0] is not None:
                deps.add(st[0])
        for w in writes:
            st = self.res.get(w)
            if st:
                if st[0] is not None:
                    deps.add(st[0])
                deps |= st[1]
        for r in after:
            st = self.res.get(r)
            if st and st[0] is not None:
                deps.add(st[0])
        o.deps = deps
        pr = CostProbe(eng)
        emit(pr)
        o.cost, o.lat, o.cls = pr.cost, pr.lat, pr.cls
        if dma is not None:
            self._sem(dma)
        for r in reads:
            st = self.res.setdefault(r, [None, set()])
            st[1].add(o.idx)
        for w in writes:
            self.res[w] = [o.idx, set()]
        o.seg = len(self.segs) - 1
        self.ops.append(o)
        self.segs[-1].append(o)

    def barrier(self):
        self.segs.append([])
        self.res = {k: v for k, v in self.res.items() if isinstance(k, tuple) and k[0] in self.PERSIST}

    def _schedule_seg(self, seg):
        ENG = self.ENG
        rem = {e: [o for o in seg if o.eng == e] for e in ENG}
        pos = {e: 0 for e in ENG}
        done = {}
        eng_free = {e: 0.0 for e in ENG}
        order = {e: [] for e in ENG}
        sched = set()
        segidx = set(o.idx for o in seg)
        n_left = len(seg)
        while n_left:
            best = None
            for e in ENG:
                lst = rem[e]
                p = pos[e]
                while p < len(lst) and lst[p].idx in sched:
                    p += 1
                pos[e] = p
                seen_dma = False
                cnt = 0
                q = p
                while q < len(lst) and cnt < self.WINDOW:
                    o = lst[q]
                    q += 1
                    if o.idx in sched:
                        continue
                    cnt += 1
                    if o.dma is not None:
                        if seen_dma:
                            continue
                        seen_dma = True
                    t = eng_free[e]
                    ok = True
                    for d in o.deps:
                        if d in segidx:
                            fd = done.get(d)
                            if fd is None:
                                ok = False
                                break
                            if fd + self.LAT > t:
                                t = fd + self.LAT
                    if not ok:
                        continue
                    pen = self.TBL if (o.cls is not None and o.cls != self.cur_cls) else 0.0
                    key = (t + pen, o.idx)
                    if best is None or key < best[0]:
                        best = (key, o, pen)
                    if t + pen <= eng_free[e]:
                        break
            assert best is not None, "scheduler deadlock"
            (t, _), o, pen = best
            if o.cls is not None:
                self.cur_cls = o.cls
            sched.add(o.idx)
            eng_free[o.eng] = t + o.cost
            done[o.idx] = t + o.cost + o.lat
            order[o.eng].append(o)
            n_left -= 1
        return order, max(eng_free.values())

    def finalize(self):
        sems = self.sems
        streams = {e: [] for e in self.ENG}
        total = {s: 0 for s in sems}
        known = {e: {} for e in self.ENG}
        est = 0.0
        orders, snaps = [], []
        for seg in self.segs:
            order, t_end = self._schedule_seg(seg)
            est += t_end
            for e in self.ENG:
                for o in order[e]:
                    if o.dma is None:
                        total[e] += 1
                        o.rec = (e, total[e])
                    else:
                        total[o.dma] += 16 * o.ndma
                        o.rec = (o.dma, total[o.dma])
            orders.append(order)
            snaps.append(dict(total))
        final = dict(total)
        for si, order in enumerate(orders):
            for e in self.ENG:
                for o in order[e]:
                    waits = []
                    need = {}
                    for d in o.deps:
                        s_, v_ = self.ops[d].rec
                        if s_.startswith(self.BACKGROUND):
                            v_ = final[s_]
                        if need.get(s_, 0) < v_:
                            need[s_] = v_
                    for s_, v_ in need.items():
                        if s_ == 'pe' and e == 'pe':
                            continue
                        if known[e].get(s_, 0) < v_:
                            waits.append((s_, v_))
                            known[e][s_] = v_
                    sem = sems[o.rec[0]]

                    def run(eh, waits=waits, o=o, sem=sem):
                        for s_, v_ in waits:
                            eh.wait_ge(sems[s_], v_)
                        r = o.emit(eh)
                        if isinstance(r, (list, tuple)):
                            assert o.dma is not None and len(r) == o.ndma
                            for x in r:
                                x.then_inc(sem, 16)
                        else:
                            assert o.dma is None or o.ndma == 1
                            r.then_inc(sem, 16 if o.dma is not None else 1)
                    streams[e].append(run)
            last = si == len(orders) - 1
            snap = {s_: v_ for s_, v_ in snaps[si].items() if last or not s_.startswith(self.BACKGROUND)}
            for e in self.ENG:
                waits = [(s_, v_) for s_, v_ in snap.items() if v_ > 0 and known[e].get(s_, 0) < v_]
                for s_, v_ in waits:
                    known[e][s_] = v_

                def runb(eh, waits=waits):
                    for s_, v_ in waits:
                        eh.wait_ge(sems[s_], v_)
                streams[e].append(runb)
        self.est_us = est
        return streams


class Bump:
    def __init__(self, nc, base, start, end):
        self.nc, self.base, self.cur, self.end = nc, base, start, end
        self.n = 0

    def take(self, name, shape, dtype):
        nbytes = int(np.prod(shape[1:])) * DTSIZE[dtype]
        nbytes = (nbytes + 31) // 32 * 32
        assert self.cur + nbytes <= self.end, (name, self.cur, nbytes, self.end)
        h = self.nc.alloc_sbuf_tensor_at(name, list(shape), dtype, offset=self.base + self.cur)
        self.cur += nbytes
        return h


def build(debug=False, stop_after=None):
    nc = bass.Bass("TRN2", target_bir_lowering=False)
    dbg_outs = []

    def dram(name, shape, dt, kind):
        return nc.dram_tensor(name, list(shape), dt, kind=kind).ap()

    x = dram("x", [TOK, D], F32, "ExternalInput")
    p_in = dram("p", [TOK, 256], F32, "ExternalInput")
    w_in = dram("w_in", [D, 3584], F32, "ExternalInput")
    w_co = dram("w_conv_out", [512, D], F32, "ExternalInput")
    w_pool = dram("w_pool", [4, 128, 256], F32, "ExternalInput")
    w_out = dram("w_out", [D, D], F32, "ExternalInput")
    w_router = dram("w_router", [D, NE], F32, "ExternalInput")
    w_eg = dram("w_exp_gate", [NE, D, D], F32, "ExternalInput")
    w_eu = dram("w_exp_up", [NE, D, D], F32, "ExternalInput")
    w_ed = dram("w_exp_down", [NE, D, D], F32, "ExternalInput")
    w_pg = dram("w_ple_gate", [D, D], F32, "ExternalInput")
    w_ple = dram("w_ple", [256, D], F32, "ExternalInput")
    b_ple = dram("b_ple_gate", [1, D], F32, "ExternalInput")
    gains = dram("gains", [5, D], F32, "ExternalInput")
    colpack_d = dram("colpack", [128, 168], F32, "ExternalInput")
    ident_d = dram("ident", [128, 128], F32, "ExternalInput")
    zeros_d = dram("zeros", [512, D], F32, "ExternalInput")
    out = dram("out", [TOK, D], F32, "ExternalOutput")
    X1 = dram("x1_scr", [TOK, D], F32, "Internal")
    H2 = dram("h2_scr", [TOK, D], BF16, "Internal")
    ACC = dram("acc_scr", [TOK, D], F32, "Internal")
    W16 = [dram("w16_%d" % m, [NE, D, D], BF16, "Internal") for m in range(3)]

    ARENA = 206 * K
    NXT = 3
    NHB = 6
    arena = nc.alloc_sbuf_tensor("arena", [128, ARENA // 4], F32)
    base = nc.lookup_mloc(arena).addr
    ps = nc.alloc_psum_tensor("ps", [128, 8, 512], F32)

    def psf(b):
        return ps[:, b, :]

    def psb(b):
        return ps[:, b, :].bitcast(BF16)

    stack = ExitStack()
    P = Prog(nc, stack)

    pers = Bump(nc, base, 0, 8 * K)
    ident_bf = pers.take("ident_bf", [128, 128], BF16)
    ident_f = pers.take("ident_f", [128, 128], F32)
    onesm = pers.take("onesm", [128, 128], F32)
    ones_row = pers.take("ones_row", [1, 128], BF16)
    bple_row = pers.take("bple_row", [1, D], BF16)
    colpack = pers.take("colpack_s", [128, 168], F32)
    wr = pers.take("wr", [128, 8, NE], BF16)
    aff_all = pers.take("aff_all", [128, 16, 32], F32)
    idxT = pers.take("idxT", [128, 2, 32], I32)
    tvT = pers.take("tvT", [128, 2, 32], F32)
    epsT = pers.take("eps", [128, 1], F32)
    mhalf = pers.take("mhalf", [128, 1], F32)
    st = pers.take("st", [128, 256], F32)
    C_BG, C_CW, C_CB, C_LG, C_LB, C_PS, C_G3 = 0, 16, 140, 144, 148, 152, 160

    def dump(name, ap, reads, eng='sp'):
        if not debug:
            return
        t = dram("dbg_" + name, list(ap.shape), ap.dtype, "ExternalOutput")
        dbg_outs.append("dbg_" + name)
        P.op('sp', lambda e: e.dma_start(out=t, in_=ap), reads=reads, dma='dbg_' + name)

    P.op('sp', lambda e: e.dma_start(out=ident_f[:], in_=ident_d), writes=['ident_f'], dma='su_ident_f')
    P.op('sp', lambda e: e.dma_start(out=colpack[:], in_=colpack_d), writes=['colpack'], dma='su_colpack')
    P.op('pool', lambda e: e.dma_start(out=ident_bf[:], in_=ident_d), writes=['ident_bf'], dma='su_ident_bf')
    P.op('pool', lambda e: e.dma_start(out=wr[:], in_=w_router.rearrange("(j p) e -> p j e", p=128)),
         writes=['wr'], dma='su_wr')
    P.op('pool', lambda e: e.dma_start(out=bple_row[:], in_=b_ple), writes=['bple_row'], dma='su_bple')
    P.op('dve', lambda e: e.memset(onesm[:], 1.0 / 512), writes=['onesm'])
    P.op('dve', lambda e: e.memset(ones_row[:], 1.0), writes=['ones_row'])
    P.op('dve', lambda e: e.memset(epsT[:], EPS), writes=['eps'])
    P.op('dve', lambda e: e.memset(mhalf[:], -0.5), writes=['mhalf'])

    def zero_acc():
        for j in range(TOK // 512):
            P.op('sp', lambda e, j=j: e.dma_start(out=ACC[j * 512:(j + 1) * 512, :], in_=zeros_d), writes=[('ACC', j)], dma='accz')

    gb = Bump(nc, base, 8 * K, 20 * K)
    g1b = gb.take("g1b", [128, D], F32)
    g2b = gb.take("g2b", [128, D], F32)
    xt_extra = gb.take("xtA2", [128, D], F32)
    mm = Bump(nc, base, 20 * K, 143 * K)
    hT = mm.take("hT", [128, 8, S], BF16)
    dd = mm.take("dd", [128, 4, S], BF16)
    WA = mm.take("WA", [128, 8, 2048], BF16)
    Wco = mm.take("Wco", [128, 4, D], BF16)
    Wpool = mm.take("Wpool", [128, 4, 256], BF16)
    Wout = mm.take("Wout", [128, 8, D], BF16)
    VW = 2080
    vpad = mm.take("vpad", [128, 4, VW], BF16)
    sa = Bump(nc, base, 143 * K, 206 * K)
    UW = 2064
    upad = sa.take("upad", [128, 4, UW], F32)
    XT = [sa.take("xtA%d" % i, [128, D], F32) for i in range(2)] + [xt_extra]
    hb = sa.take("hb", [128, NHB, D], BF16)
    sgt = [sa.take("sgt%d" % i, [128, 512], F32) for i in range(2)]
    tA = sa.take("tA", [128, 528], F32)
    tB = sa.take("tB", [128, 528], F32)
    sb = Bump(nc, base, 143 * K, 206 * K)
    c_act = sb.take("c_act", [128, 4, S], BF16)
    conv_blk = sb.take("conv_blk", [128, 4, 512], F32)
    sq = sb.take("sq", [128, 4, 512], F32)
    sb.cur = 143 * K + 33024
    dg = [sb.take("dg%d" % i, [128, 31, 128], BF16) for i in range(2)]
    m2t = sb.take("m2t", [128, 512], F32)
    vart = sb.take("vart", [128, 512], F32)
    rstdt = sb.take("rstdt", [128, 512], F32)
    nbt = sb.take("nbt", [128, 512], F32)
    tt = [sb.take("tt%d" % i, [128, 512], F32) for i in range(2)]
    cacc = sb.take("cacc", [128, 512], F32)
    sc = Bump(nc, base, 143 * K + 16 * K, 206 * K)
    sgc = [sc.take("sgc%d" % i, [128, 512], F32) for i in range(2)]
    sgp = [sc.take("sgp%d" % i, [128, 512], F32) for i in range(2)]
    m1t = [sc.take("m1t%d" % i, [128, 512], F32) for i in range(2)]
    m2c = [sc.take("m2c%d" % i, [128, 512], F32) for i in range(2)]
    mgs = [sc.take("mg%d" % i, [128, 8, 512], BF16) for i in range(2)]
    XTC = [sc.take("xtC%d" % i, [128, D], F32) for i in range(2)]
    x1t = [sc.take("x1t0", [128, D], F32)]
    sv = Bump(nc, base, mm.cur - 4 * VW * 2 - 0, mm.cur)
    x1t.append(sv.take("x1t1", [128, D], F32))
    XTC.append(sv.take("xtC2", [128, D], F32))
    XTC.append(xt_extra)
    h2t = [sv.take("h2t%d" % i, [128, D], BF16) for i in range(2)]
    h2T = [sv.take("h2T%d" % i, [128, D], BF16) for i in range(2)]
    ext = sv.take("ext", [128, 32], F32)

    P.op('sp', lambda e: e.dma_start(out=g1b[:], in_=gains[0, :].partition_broadcast(128)), writes=['g1b'], dma='su_g1b')
    P.op('sp', lambda e: e.dma_start(out=g2b[:], in_=gains[1, :].partition_broadcast(128)), writes=['g2b'], dma='su_g2b')

    def load_w(dst, src, key, nk, eng='pool'):
        def emit(e):
            return [e.dma_start(out=dst[:, k, :], in_=src[k * 128:(k + 1) * 128, :]) for k in range(nk)]
        P.op(eng, emit, writes=[key], dma='w_' + key if isinstance(key, str) else 'w_' + "_".join(map(str, key)), ndma=nk)

    load_w(Wco, w_co, 'Wco', 4)
    P.op('pool', lambda e: [e.dma_start(out=Wpool[:, g, :], in_=w_pool[g, :, :]) for g in range(4)],
         writes=['Wpool'], dma='w_Wpool', ndma=4)
    load_w(Wout, w_out, 'Wout', 8)

    cw = lambda c: colpack[:, c:c + 1]

    pool_pow = [False]

    def rms_rstd(src_ap, junk_ap, reads, jkeys):
        c0, c1, c2 = P.col(), P.col(), P.col()
        P.op('dve', lambda e: e.memset(st[:, c0:c0 + 1], 0.0), writes=[('st', c0)])
        P.op('act', lambda e: e.activation(out=junk_ap, in_=src_ap, func=AF.Square, accum_out=st[:, c0:c0 + 1]),
             reads=list(reads) + [('st', c0)], writes=[('st', c0)] + list(jkeys))
        if pool_pow[0]:
            P.op('pool', lambda e: e.tensor_scalar(out=st[:, c1:c1 + 1], in0=st[:, c0:c0 + 1], scalar1=1.0 / D, scalar2=EPS, op0=ALU.mult, op1=ALU.add),
                 reads=[('st', c0)], writes=[('st', c1)])
            P.op('pool', lambda e: e.tensor_tensor(out=st[:, c2:c2 + 1], in0=st[:, c1:c1 + 1], in1=mhalf[:, 0:1], op=ALU.pow),
                 reads=[('st', c1), 'mhalf'], writes=[('st', c2)])
            return c2
        P.op('act', lambda e: e.activation(out=st[:, c1:c1 + 1], in_=st[:, c0:c0 + 1], func=AF.Sqrt,
                                           bias=epsT[:, 0:1], scale=1.0 / D),
             reads=[('st', c0), 'eps'], writes=[('st', c1)])
        P.op('dve', lambda e: e.reciprocal(out=st[:, c2:c2 + 1], in_=st[:, c1:c1 + 1]),
             reads=[('st', c1)], writes=[('st', c2)])
        return c2

    cntA = [0]
    cntC = [0]
    hbmap = {}
    GA_early = [('hT', b_, jp_) for b_ in range(4) for jp_ in range(4)] + [('hb', h_) for h_ in range(NHB)]
    GA_all = GA_early + [('v', b_, i_) for b_ in range(4) for i_ in range(4)] + [('d', b_, i_) for b_ in range(4) for i_ in range(4)]
    GC = [('ca', 3, i_) for i_ in range(4)]
    VKEYS = [('v', b_, i_) for b_ in range(4) for i_ in range(4)] + [('vz', i_, z_) for i_ in range(4) for z_ in range(2)]
    W32 = (w_eg, w_eu, w_ed)
    conv_list = [(e_, m) for e_ in range(NE) for m in range(3)]
    conv_pos = [0]

    def convert_weights(n, dep=None):
        for _ in range(n):
            if conv_pos[0] >= len(conv_list):
                return
            e_, m = conv_list[conv_pos[0]]
            conv_pos[0] += 1
            P.op('pool', lambda e, e_=e_, m=m: [e.dma_start(out=W16[m][e_, hh * 512:(hh + 1) * 512, :], in_=W32[m][e_, hh * 512:(hh + 1) * 512, :], max_dma_last_dim=2048) for hh in range(2)],
                 after=([dep] if dep is not None else []), writes=[('w16', e_, m)], dma='cv%d' % (conv_pos[0] % 4), ndma=2)

    def tileA(s, b, tl):
        r0 = s * S
        i = 4 * b + tl
        slot = cntA[0] % NXT
        hs = cntA[0] % NHB
        cntA[0] += 1
        hbmap[(s, b, tl)] = hs
        xt = XT[slot]
        src = x[r0 + i * 128: r0 + (i + 1) * 128, :]
        P.op('sp', lambda e: e.dma_start(out=xt[:], in_=src), writes=[('xtA', slot)], dma='xtA%d' % slot)
        c = rms_rstd(xt[:], hb[:, hs, :], [('xtA', slot)], [('hb', hs)])
        P.op('dve', lambda e: e.scalar_tensor_tensor(
            out=hb[:, hs, :], in0=xt[:], scalar=st[:, c:c + 1], in1=g1b[:], op0=ALU.mult, op1=ALU.mult),
            reads=[('xtA', slot), ('st', c), 'g1b'], writes=[('hb', hs)])

    def transA(s, b, jp):
        bk = P.bank()
        hss = [hbmap[(s, b, tl)] for tl in range(4)]

        def tr(e):
            r = None
            for jj in range(2):
                for tl in range(4):
                    r = e.transpose(out=psb(bk)[:, (jj * 4 + tl) * 128:(jj * 4 + tl + 1) * 128],
                                    in_=hb[:, hss[tl], (2 * jp + jj) * 128:(2 * jp + jj + 1) * 128], identity=ident_bf[:])
            return r
        P.op('pe', tr, reads=[('hb', t_) for t_ in hss] + ['ident_bf'], writes=[('ps', bk)])
        src = psb(bk).rearrange("p (j t) -> p j t", j=2)
        dst = hT[:, 2 * jp:2 * jp + 2, b * 512:(b + 1) * 512]
        if jp % 2 == 0:
            P.op('act', lambda e: e.activation(out=dst, in_=src, func=AF.Copy), reads=[('ps', bk)], writes=[('hT', b, jp)])
        else:
            P.op('dve', lambda e: e.tensor_copy(out=dst, in_=src), reads=[('ps', bk)], writes=[('hT', b, jp)])

    def mm_hT(bk, W, c0, b):
        def emit(e):
            r = None
            for k in range(8):
                r = e.matmul(psf(bk), lhsT=W[:, k, c0:c0 + 128], rhs=hT[:, k, b * 512:(b + 1) * 512], start=(k == 0), stop=(k == 7))
            return r
        return emit

    def pairA(b, i):
        hTr = [('hT', b, jp) for jp in range(4)]
        ba, bb_ = P.bank(), P.bank()
        P.op('pe', mm_hT(ba, WA, i * 128, b), reads=hTr + ['WA'], writes=[('ps', ba)])
        P.op('pe', mm_hT(bb_, WA, 512 + i * 128, b), reads=hTr + ['WA'], writes=[('ps', bb_)])
        sl = (4 * b + i) % 2
        P.op('act', lambda e: e.activation(out=sgt[sl][:], in_=psf(bb_), func=AF.Sigmoid), reads=[('ps', bb_)], writes=[('sgt', sl)])
        P.op('dve', lambda e: e.tensor_tensor(out=vpad[:, i, 15 + b * 512: 15 + (b + 1) * 512], in0=psf(ba), in1=sgt[sl][:], op=ALU.mult),
             reads=[('ps', ba), ('sgt', sl)], writes=[('v', b, i)])

    def uA(b, i):
        hTr = [('hT', b, jp) for jp in range(4)]
        bu = P.bank()
        P.op('pe', mm_hT(bu, WA, 1024 + i * 128, b), reads=hTr + ['WA'], writes=[('ps', bu)])
        dst = upad[:, i, 8 + b * 512: 8 + (b + 1) * 512]
        if i % 2 == 0:
            P.op('act', lambda e: e.activation(out=dst, in_=psf(bu), func=AF.Copy), reads=[('ps', bu)], writes=[('u', b, i)])
        else:
            P.op('dve', lambda e: e.tensor_copy(out=dst, in_=psf(bu)), reads=[('ps', bu)], writes=[('u', b, i)])

    def u_ap(i, lo, n):
        return upad[:, i, 8 + lo: 8 + lo + n]

    def dve_add(o, a, b_, reads, writes):
        P.op('dve', lambda e: e.tensor_tensor(out=o, in0=a, in1=b_, op=ALU.add), reads=reads, writes=writes)

    def pool_fin(bb, i, wbuf, wkey):
        T0 = bb * 512
        h = 1 << i
        ur = [('u', b2, i) for b2 in range(max(0, bb - 1), min(3, bb + 1) + 1)] + [('uz', i, 0), ('uz', i, 1)]
        P.op('dve', lambda e: e.scalar_tensor_tensor(out=dd[:, i, T0:T0 + 512], in0=wbuf[:, 0:512], scalar=1.0 / (2 * h),
                                                     in1=u_ap(i, T0, 512), op0=ALU.mult, op1=ALU.subtract),
             reads=[wkey] + ur, writes=[('d', bb, i)])
        cols = []
        if bb == 0:
            cols += [(t, t + h) for t in range(0, h)]
        if bb == 3:
            cols += [(t, S - t + h) for t in range(S - h + 1, S)]
        if cols:
            def emit(e):
                r = None
                for t, cnt in cols:
                    r = e.scalar_tensor_tensor(out=dd[:, i, t:t + 1], in0=wbuf[:, t - T0:t - T0 + 1], scalar=1.0 / cnt,
                                               in1=u_ap(i, t, 1), op0=ALU.mult, op1=ALU.subtract)
                return r
            P.op('dve', emit, reads=[wkey] + ur, writes=[('d', bb, i)])

    def pool_block(bb):
        T0 = bb * 512
        ur = lambda i: [('u', b2, i) for b2 in range(max(0, bb - 1), min(3, bb + 1) + 1)] + [('uz', i, 0), ('uz', i, 1)]
        dve_add(tA[:, 0:512], u_ap(0, T0 - 1, 512), u_ap(0, T0, 512), ur(0), ['tA'])
        pool_fin(bb, 0, tA, 'tA')
        dve_add(tB[:, 0:514], u_ap(1, T0 - 2, 514), u_ap(1, T0 - 1, 514), ur(1), ['tB'])
        dve_add(tA[:, 0:512], tB[:, 0:512], tB[:, 2:514], ['tB'], ['tA'])
        pool_fin(bb, 1, tA, 'tA')
        dve_add(tA[:, 0:518], u_ap(2, T0 - 4, 518), u_ap(2, T0 - 3, 518), ur(2), ['tA'])
        dve_add(tB[:, 0:516], tA[:, 0:516], tA[:, 2:518], ['tA'], ['tB'])
        dve_add(tA[:, 0:512], tB[:, 0:512], tB[:, 4:516], ['tB'], ['tA'])
        pool_fin(bb, 2, tA, 'tA')
        dve_add(tA[:, 0:526], u_ap(3, T0 - 8, 526), u_ap(3, T0 - 7, 526), ur(3), ['tA'])
        dve_add(tB[:, 0:524], tA[:, 0:524], tA[:, 2:526], ['tA'], ['tB'])
        dve_add(tA[:, 0:520], tB[:, 0:520], tB[:, 4:524], ['tB'], ['tA'])
        dve_add(tB[:, 0:512], tA[:, 0:512], tA[:, 8:520], ['tA'], ['tB'])
        pool_fin(bb, 3, tB, 'tB')

    def zpadA(i):
        P.op('dve', lambda e: e.memset(vpad[:, i, 0:15], 0.0), writes=[('vz', i, 0)])
        P.op('dve', lambda e: e.memset(vpad[:, i, 15 + S:VW], 0.0), writes=[('vz', i, 1)])
        P.op('dve', lambda e: e.memset(upad[:, i, 0:8], 0.0), writes=[('uz', i, 0)])
        P.op('dve', lambda e: e.memset(upad[:, i, 8 + S:UW], 0.0), writes=[('uz', i, 1)])

    TP = 8

    def convB(b, i):
        sl = (4 * b + i) % 2
        P.op('dve', lambda e: e.tensor_tensor(
            out=dg[sl][:, 0:TP, :], in0=ident_bf[:].unsqueeze(1).broadcast_to([128, TP, 128]),
            in1=colpack[:, C_CW + i * 31: C_CW + i * 31 + TP].unsqueeze(2).broadcast_to([128, TP, 128]),
            op=ALU.mult), reads=['ident_bf', 'colpack'], writes=[('dg', sl)], after=GA_early)
        bk = P.bank()

        def conv(e):
            r = None
            for k in range(TP):
                r = e.matmul(psf(bk), lhsT=dg[sl][:, k, :], rhs=vpad[:, i, b * 512 + k: b * 512 + k + 512],
                             start=(k == 0), stop=(k == TP - 1))
            return r
        P.op('pe', conv, reads=[('dg', sl)] + VKEYS, writes=[('ps', bk)])
        accs = [(conv_blk[:, i, :], ('cb', i)), (cacc[:], 'cacc')]
        started = [False, False]
        for n, k in enumerate(range(TP, 31)):
            dve_tap(b, i, k, accs[n % 2], started[n % 2])
            started[n % 2] = True
        P.op('dve', lambda e: e.tensor_tensor(out=conv_blk[:, i, :], in0=conv_blk[:, i, :], in1=cacc[:], op=ALU.add),
             reads=[('cb', i), 'cacc'], writes=[('cb', i)])
        P.op('dve', lambda e: e.tensor_tensor(out=conv_blk[:, i, :], in0=conv_blk[:, i, :], in1=psf(bk), op=ALU.add),
             reads=[('cb', i), ('ps', bk)], writes=[('cb', i)])
        P.op('act', lambda e: e.activation(out=sq[:, i, :], in_=conv_blk[:, i, :], func=AF.Square),
             reads=[('cb', i)], writes=[('sq', i)], after=GA_all)

    def dve_tap(b, i, k, acc, started):
        acc_ap, key = acc
        src = vpad[:, i, b * 512 + k: b * 512 + k + 512]
        wk = cw(C_CW + i * 31 + k)
        if not started:
            if key == 'cacc':
                P.op('dve', lambda e: e.tensor_scalar(out=acc_ap, in0=src, scalar1=wk, scalar2=None, op0=ALU.mult),
                     reads=VKEYS + ['colpack'], writes=[key], after=GA_all)
            else:
                P.op('dve', lambda e: e.tensor_scalar(out=acc_ap, in0=src, scalar1=wk, scalar2=cw(C_CB + i), op0=ALU.mult, op1=ALU.add),
                     reads=VKEYS + ['colpack'], writes=[key], after=GA_all)
        else:
            P.op('dve', lambda e: e.scalar_tensor_tensor(out=acc_ap, in0=src, scalar=wk, in1=acc_ap, op0=ALU.mult, op1=ALU.add),
                 reads=VKEYS + ['colpack', key], writes=[key])

    def statB(bk, src, keys):
        def emit(e):
            r = None
            for i in range(4):
                r = e.matmul(psf(bk), lhsT=onesm[:], rhs=src[:, i, :], start=(i == 0), stop=(i == 3))
            return r
        P.op('pe', emit, reads=keys + ['onesm'], writes=[('ps', bk)])

    def lnB(b):
        bm, bq = P.bank(), P.bank()
        statB(bm, conv_blk, [('cb', i) for i in range(4)])
        statB(bq, sq, [('sq', i) for i in range(4)])
        P.op('act', lambda e: e.activation(out=m2t[:], in_=psf(bm), func=AF.Square), reads=[('ps', bm)], writes=['m2t'], after=GA_all)
        P.op('dve', lambda e: e.tensor_tensor(out=vart[:], in0=psf(bq), in1=m2t[:], op=ALU.subtract),
             reads=[('ps', bq), 'm2t'], writes=['vart'], after=GA_all)
        P.op('dve', lambda e: e.tensor_scalar(out=vart[:], in0=vart[:], scalar1=0.0, scalar2=EPS, op0=ALU.max, op1=ALU.add),
             reads=['vart'], writes=['vart'])
        P.op('act', lambda e: e.activation(out=rstdt[:], in_=vart[:], func=AF.Sqrt), reads=['vart'], writes=['rstdt'], after=GA_all)
        P.op('dve', lambda e: e.reciprocal(out=rstdt[:], in_=rstdt[:]), reads=['rstdt'], writes=['rstdt'])
        P.op('dve', lambda e: e.scalar_tensor_tensor(out=nbt[:], in0=psf(bm), scalar=-1.0, in1=rstdt[:], op0=ALU.mult, op1=ALU.mult),
             reads=[('ps', bm), 'rstdt'], writes=['nbt'], after=GA_all)
        for i in range(4):
            lnB_chunk(b, i)

    def lnB_chunk(b, i):
        sl = i % 2
        P.op('dve', lambda e: e.tensor_tensor(out=tt[sl][:], in0=conv_blk[:, i, :], in1=rstdt[:], op=ALU.mult),
             reads=[('cb', i), 'rstdt'], writes=[('tt', sl)], after=GA_all)
        P.op('dve', lambda e: e.tensor_tensor(out=tt[sl][:], in0=tt[sl][:], in1=nbt[:], op=ALU.add),
             reads=[('tt', sl), 'nbt'], writes=[('tt', sl)])
        P.op('act', lambda e: e.activation(out=c_act[:, i, b * 512:(b + 1) * 512], in_=tt[sl][:], func=AF.Silu,
                                           bias=cw(C_LB + i), scale=cw(C_LG + i)),
             reads=[('tt', sl), 'colpack'], writes=[('ca', b, i)], after=GA_all)

    def gateC(b, j):
        blk = slice(b * 512, (b + 1) * 512)
        byc, byp, bgc, bgp = P.bank(), P.bank(), P.bank(), P.bank()

        def yc(e):
            r = None
            for i in range(4):
                r = e.matmul(psf(byc), lhsT=Wco[:, i, j * 128:(j + 1) * 128], rhs=c_act[:, i, blk], start=(i == 0), stop=(i == 3))
            return r
        P.op('pe', yc, reads=['Wco'] + [('ca', b, i_) for i_ in range(4)], writes=[('ps', byc)])
        g = j // 2
        P.op('pe', lambda e: e.matmul(psf(byp), lhsT=Wpool[:, g, (j % 2) * 128:(j % 2 + 1) * 128], rhs=dd[:, g, blk], start=True, stop=True),
             reads=['Wpool', ('d', b, g)], writes=[('ps', byp)])
        P.op('pe', mm_hT(bgc, WA, j * 128, b), reads=['WA'] + [('hT', b, jp_) for jp_ in range(4)], writes=[('ps', bgc)])
        P.op('pe', mm_hT(bgp, WA, 1024 + j * 128, b), reads=['WA'] + [('hT', b, jp_) for jp_ in range(4)], writes=[('ps', bgp)])
        sl = j % 2
        P.op('act', lambda e: e.activation(out=sgc[sl][:], in_=psf(bgc), func=AF.Sigmoid, bias=cw(C_BG + j)),
             reads=[('ps', bgc), 'colpack'], writes=[('sgc', sl)], after=GC)
        P.op('act', lambda e: e.activation(out=sgp[sl][:], in_=psf(bgp), func=AF.Sigmoid, bias=cw(C_BG + 8 + j)),
             reads=[('ps', bgp), 'colpack'], writes=[('sgp', sl)], after=GC)
        P.op('dve', lambda e: e.tensor_tensor(out=m1t[sl][:], in0=psf(byc), in1=sgc[sl][:], op=ALU.mult),
             reads=[('ps', byc), ('sgc', sl)], writes=[('m1t', sl)], after=GC)
        P.op('dve', lambda e: e.scalar_tensor_tensor(out=m2c[sl][:], in0=psf(byp), scalar=cw(C_PS + j), in1=sgp[sl][:], op0=ALU.mult, op1=ALU.mult),
             reads=[('ps', byp), ('sgp', sl), 'colpack'], writes=[('m2c', sl)], after=GC)
        mg = mgs[b % 2]
        P.op('dve', lambda e: e.tensor_tensor(out=mg[:, j, :], in0=m1t[sl][:], in1=m2c[sl][:], op=ALU.add),
             reads=[('m1t', sl), ('m2c', sl)], writes=[('mg', b % 2, j)], after=GC)

    def woC(b, slot, xs, tl, h):
        bo = P.bank()
        xt = XTC[xs]
        mg = mgs[b % 2]

        def wo(e):
            r = None
            for k in range(8):
                r = e.matmul(psf(bo), lhsT=mg[:, k, tl * 128:(tl + 1) * 128], rhs=Wout[:, k, h * 512:(h + 1) * 512],
                             start=(k == 0), stop=(k == 7))
            return r
        P.op('pe', wo, reads=[('mg', b % 2, j) for j in range(8)] + ['Wout'], writes=[('ps', bo)])
        P.op('dve', lambda e: e.tensor_tensor(out=x1t[slot][:, h * 512:(h + 1) * 512], in0=psf(bo), in1=xt[:, h * 512:(h + 1) * 512], op=ALU.add),
             reads=[('ps', bo), ('xtC', xs)], writes=[('x1t', slot, h)], after=GC)

    def loadC(s, i):
        xs = i % 4
        rows = slice(s * S + i * 128, s * S + (i + 1) * 128)
        P.op('sp', lambda e: e.dma_start(out=XTC[xs][:], in_=x[rows, :]), writes=[('xtC', xs)], dma='xtC%d' % xs, after=GC)

    def tileC(s, b, tl):
        r0 = s * S
        i = 4 * b + tl
        slot = cntC[0] % 2
        xs = i % 4
        cntC[0] += 1
        xt = XTC[xs]
        rows = slice(r0 + i * 128, r0 + (i + 1) * 128)
        if i + 3 < 16:
            loadC(s, i + 3)
        woC(b, slot, xs, tl, 0)
        woC(b, slot, xs, tl, 1)
        xk = [('x1t', slot, 0), ('x1t', slot, 1)]
        P.op('sp', lambda e: e.dma_start(out=X1[rows, :], in_=x1t[slot][:]), reads=xk, writes=[('X1', s, i)], dma='x1st%d' % slot)
        c = rms_rstd(x1t[slot][:], h2t[slot][:], xk, [('h2t', slot)])
        P.op('dve', lambda e: e.scalar_tensor_tensor(out=h2t[slot][:], in0=x1t[slot][:], scalar=st[:, c:c + 1], in1=g2b[:], op0=ALU.mult, op1=ALU.mult),
             reads=xk + [('st', c), 'g2b'], writes=[('h2t', slot)])
        P.op('sp', lambda e: e.dma_start(out=H2[rows, :], in_=h2t[slot][:]), reads=[('h2t', slot)], writes=[('H2', s, i)], dma='h2st%d' % slot)
        bt = P.bank()

        def tr2(e):
            r = None
            for k in range(8):
                r = e.transpose(out=psb(bt)[:, k * 128:(k + 1) * 128], in_=h2t[slot][:, k * 128:(k + 1) * 128], identity=ident_bf[:])
            return r
        P.op('pe', tr2, reads=[('h2t', slot), 'ident_bf'], writes=[('ps', bt)])
        P.op('act', lambda e: e.activation(out=h2T[slot][:], in_=psb(bt), func=AF.Copy), reads=[('ps', bt)], writes=[('h2T', slot)])
        bl = P.bank()

        def rt(e):
            r = None
            for k in range(8):
                r = e.matmul(psf(bl)[:, 0:NE], lhsT=h2T[slot][:, k * 128:(k + 1) * 128], rhs=wr[:, k, :], start=(k == 0), stop=(k == 7))
            return r
        P.op('pe', rt, reads=[('h2T', slot), 'wr'], writes=[('ps', bl)])
        c0, c1, c2, c3 = P.col(), P.col(), P.col(), P.col()
        P.op('dve', lambda e: e.reduce_max(out=st[:, c0:c0 + 1], in_=psf(bl)[:, 0:NE], axis=AX.X), reads=[('ps', bl)], writes=[('st', c0)])
        P.op('dve', lambda e: e.tensor_scalar(out=st[:, c1:c1 + 1], in0=st[:, c0:c0 + 1], scalar1=-1.0, scalar2=None, op0=ALU.mult),
             reads=[('st', c0)], writes=[('st', c1)])
        P.op('dve', lambda e: e.memset(st[:, c2:c2 + 1], 0.0), writes=[('st', c2)])
        es = slot
        P.op('act', lambda e: e.activation(out=ext[:, es * 16:(es + 1) * 16], in_=psf(bl)[:, 0:NE], func=AF.Exp,
                                           bias=st[:, c1:c1 + 1], accum_out=st[:, c2:c2 + 1]),
             reads=[('ps', bl), ('st', c1), ('st', c2)], writes=[('ext', es), ('st', c2)])
        P.op('dve', lambda e: e.reciprocal(out=st[:, c3:c3 + 1], in_=st[:, c2:c2 + 1]), reads=[('st', c2)], writes=[('st', c3)])
        P.op('dve', lambda e: e.tensor_scalar(out=aff_all[:, i, s * 16:(s + 1) * 16], in0=ext[:, es * 16:(es + 1) * 16],
                                              scalar1=st[:, c3:c3 + 1], scalar2=None, op0=ALU.mult),
             reads=[('ext', es), ('st', c3)], writes=[('aff', s, i)])
        if s == 0 and i == 0:
            dump("x1t0", x1t[slot][:], xk)

    stopped = False
    for s in range(NSEQ):
        load_w(WA[:, :, 0:1536], w_in[:, 0:1536], 'WA', 8)
        for i in range(4):
            zpadA(i)
        for b in range(4):
            for tl in range(4):
                tileA(s, b, tl)
            for jp in range(4):
                transA(s, b, jp)
            for i in range(4):
                pairA(b, i)
            for i in range(4):
                uA(b, i)
            convert_weights(2, ('u', b, 3))
            if b >= 1:
                pool_block(b - 1)
        pool_block(3)
        if s == 0:
            dump("hT", hT[:], [('hT', b, jp) for b in range(4) for jp in range(4)])
            dump("vpad", vpad[:], [('v', b, i) for b in range(4) for i in range(4)])
            dump("upad", upad[:], [('u', b, i) for b in range(4) for i in range(4)])
            dump("dd", dd[:], [('d', b, i) for b in range(4) for i in range(4)])
        if stop_after == 'A':
            P.barrier()
            stopped = True
            break
        load_w(WA, w_in[:, 1536:3584], 'WA', 8)
        if s == 1:
            zero_acc()
        for b in range(4):
            for i in range(4):
                convB(b, i)
            convert_weights(2, ('cb', 3))
            lnB(b)
        if s == 0:
            dump("c_act", c_act[:], [('ca', b, i) for b in range(4) for i in range(4)])
        if stop_after == 'B':
            P.barrier()
            stopped = True
            break
        for i in range(3):
            loadC(s, i)
        for b in range(4):
            for j in range(8):
                gateC(b, j)
            convert_weights(2, ('mg', b % 2, 7))
            if s == 0 and b == 0:
                dump("mg", mgs[0][:], [('mg', 0, j) for j in range(8)])
            for tl in range(4):
                tileC(s, b, tl)
        P.barrier()
        if stop_after == 'C':
            stopped = True
            break
    if stopped:
        return finish(nc, P, stack, dbg_outs)
    dump("aff_all", aff_all[:], [])

    em = Bump(nc, base, 8 * K, 206 * K)
    WE = [[em.take("we%d_%d" % (sl, m), [128, 8, D], BF16) for m in range(3)] for sl in range(2)]
    xg = [em.take("xg%d" % sl, [128, 4, D], BF16) for sl in range(2)]
    xgT = em.take("xgT", [128, 8, 512], BF16)
    hidT = em.take("hidT", [128, 8, 512], BF16)
    sge = [em.take("sge%d" % i, [128, 512], F32) for i in range(2)]
    ye = [em.take("ye%d" % i, [128, D], F32) for i in range(2)]
    affT = em.take("affT", [32, S], F32)
    tv = em.take("tv", [32, CAP], F32)
    ti = em.take("ti", [32, CAP], U32)
    tif = em.take("tif", [32, CAP], F32)
    idxf = em.take("idxf", [128, 2, 32], F32)
    wtop = Bump(nc, base, 186 * K, 206 * K)
    Wpg = wtop.take("Wpg", [128, 8, D], BF16)
    Wple = wtop.take("Wple", [128, 2, D], BF16)

    def load_expert(e_):
        sl = e_ % 2
        for m in range(3):
            for k2 in range(2):
                def emit(e, m=m, k2=k2):
                    return [e.dma_start(out=WE[sl][m][:, k, :], in_=W16[m][e_, k * 128:(k + 1) * 128, :]) for k in range(4 * k2, 4 * k2 + 4)]
                P.op('sp', emit, reads=[('w16', e_, m)], writes=[('we', sl, m, k) for k in range(4 * k2, 4 * k2 + 4)],
                     dma='we%d_%d_%d' % (sl, m, k2), ndma=4)

    load_expert(0)
    load_expert(1)

    def affT_blk(i4):
        bk = P.bank()

        def tra(e):
            r = None
            for q in range(4):
                r = e.matmul(psf(bk)[0:32, q * 128:(q + 1) * 128], lhsT=aff_all[:, i4 * 4 + q, :], rhs=ident_f[:], start=True, stop=True)
            return r
        P.op('pe', tra, reads=['ident_f'], writes=[('ps', bk)])
        P.op('act', lambda e: e.activation(out=affT[:, i4 * 512:(i4 + 1) * 512], in_=psf(bk)[0:32, :], func=AF.Copy),
             reads=[('ps', bk)], writes=[('affT', i4)])
    for i4 in range(4):
        affT_blk(i4)
    dump("affT", affT[:], [('affT', i4) for i4 in range(4)] + ['affTw'])

    def topk_round(r_):
        c8 = slice(r_ * 8, (r_ + 1) * 8)
        P.op('dve', lambda e: e.max(out=tv[:, c8], in_=affT[:]), reads=['affTw'] + [('affT', i4) for i4 in range(4)], writes=[('tv', r_)])
        P.op('dve', lambda e: e.max_index(out=ti[:, c8], in_max=tv[:, c8], in_values=affT[:]),
             reads=[('tv', r_), 'affTw'], writes=[('ti', r_)])
        P.op('dve', lambda e: e.match_replace(out=affT[:], in_to_replace=tv[:, c8], in_values=affT[:], imm_value=-1.0),
             reads=[('tv', r_), ('ti', r_)], writes=['affTw'])
    for r_ in range(CAP // 8):
        topk_round(r_)
    allr = [('tv', r_) for r_ in range(CAP // 8)] + [('ti', r_) for r_ in range(CAP // 8)]
    P.op('dve', lambda e: e.tensor_copy(out=tif[:], in_=ti[:]), reads=allr, writes=['tif'])
    bi, bv = P.bank(), P.bank()

    def trix(bk, src):
        def emit(e):
            r = None
            for hh in range(2):
                r = e.matmul(psf(bk)[:, hh * 32:(hh + 1) * 32], lhsT=src[:, hh * 128:(hh + 1) * 128], rhs=ident_f[0:32, 0:32], start=True, stop=True)
            return r
        return emit
    P.op('pe', trix(bi, tif), reads=['tif', 'ident_f'], writes=[('ps', bi)])
    P.op('pe', trix(bv, tv), reads=allr + ['ident_f'], writes=[('ps', bv)])
    P.op('dve', lambda e: e.tensor_copy(out=idxf[:], in_=psf(bi)[:, 0:64].rearrange("p (h c) -> p h c", h=2)), reads=[('ps', bi)], writes=['idxf'])
    P.op('dve', lambda e: e.tensor_scalar(out=idxf[:, :, 16:32], in0=idxf[:, :, 16:32], scalar1=float(S), scalar2=None, op0=ALU.add),
         reads=['idxf'], writes=['idxf'])
    P.op('dve', lambda e: e.tensor_copy(out=idxT[:], in_=idxf[:]), reads=['idxf'], writes=['idxT'])
    P.op('act', lambda e: e.activation(out=tvT[:], in_=psf(bv)[:, 0:64].rearrange("p (h c) -> p h c", h=2), func=AF.Copy),
         reads=[('ps', bv)], writes=['tvT'])
    dump("idxT", idxT[:], ['idxT'])
    dump("tvT", tvT[:], ['tvT'])
    if stop_after == 'R':
        return finish(nc, P, stack, dbg_outs)

    def gather(e_):
        sl = e_ % 2

        def emit(e):
            r = []
            for s_ in range(2):
                for hh in range(2):
                    q = s_ * 2 + hh
                    r.append(e.indirect_dma_start(out=xg[sl][:, q, :], out_offset=None, in_=H2,
                                                  in_offset=bass.IndirectOffsetOnAxis(ap=idxT[:, hh, s_ * 16 + e_: s_ * 16 + e_ + 1], axis=0)))
            return r
        P.op('pool', emit, reads=['idxT'], writes=[('xg', sl)], dma='xg%d' % sl, ndma=4)

    def trgE(sl, kp):
        bk = P.bank()

        def trg(e):
            r = None
            for kk in range(2):
                for q in range(4):
                    r = e.transpose(out=psb(bk)[:, kk * 512 + q * 128: kk * 512 + (q + 1) * 128],
                                    in_=xg[sl][:, q, (2 * kp + kk) * 128:(2 * kp + kk + 1) * 128], identity=ident_bf[:])
            return r
        P.op('pe', trg, reads=[('xg', sl), 'ident_bf'], writes=[('ps', bk)])
        src = psb(bk).rearrange("p (k c) -> p k c", k=2)
        if kp % 2 == 0:
            P.op('act', lambda e: e.activation(out=xgT[:, 2 * kp:2 * kp + 2, :], in_=src, func=AF.Copy), reads=[('ps', bk)], writes=[('xgT', kp)])
        else:
            P.op('dve', lambda e: e.tensor_copy(out=xgT[:, 2 * kp:2 * kp + 2, :], in_=src), reads=[('ps', bk)], writes=[('xgT', kp)])

    def ffn1(bk, W, f, reads):
        def emit(e):
            r = None
            for k in range(8):
                r = e.matmul(psf(bk), lhsT=W[:, k, f * 128:(f + 1) * 128], rhs=xgT[:, k, :], start=(k == 0), stop=(k == 7))
            return r
        P.op('pe', emit, reads=reads, writes=[('ps', bk)])

    def hidE(sl, f):
        Wg, Wu, Wd = WE[sl]
        xr = [('xgT', kp) for kp in range(4)]
        bg, bu = P.bank(), P.bank()
        ffn1(bg, Wg, f, xr + [('we', sl, 0, k) for k in range(8)])
        ffn1(bu, Wu, f, xr + [('we', sl, 1, k) for k in range(8)])
        s2 = f % 2
        P.op('act', lambda e: e.activation(out=sge[s2][:], in_=psf(bg), func=AF.Silu), reads=[('ps', bg)], writes=[('sge', s2)])
        P.op('dve', lambda e: e.tensor_tensor(out=hidT[:, f, :], in0=psf(bu), in1=sge[s2][:], op=ALU.mult),
             reads=[('ps', bu), ('sge', s2)], writes=[('hidT', f)])

    def downE(e_, sl, q, h):
        Wd = WE[sl][2]
        s_, hh = q // 2, q % 2
        ys = q % 2
        hr = [('hidT', f) for f in range(8)]
        bk = P.bank()

        def dn(e):
            r = None
            for f in range(8):
                r = e.matmul(psf(bk), lhsT=hidT[:, f, q * 128:(q + 1) * 128], rhs=Wd[:, f, h * 512:(h + 1) * 512], start=(f == 0), stop=(f == 7))
            return r
        P.op('pe', dn, reads=hr + [('we', sl, 2, k) for k in range(8)], writes=[('ps', bk)])
        sc_ap = tvT[:, hh, s_ * 16 + e_: s_ * 16 + e_ + 1]
        if h == 0:
            P.op('act', lambda e: e.activation(out=ye[ys][:, 0:512], in_=psf(bk), func=AF.Copy, scale=sc_ap),
                 reads=[('ps', bk), 'tvT'], writes=[('ye', ys, 0)])
        else:
            P.op('dve', lambda e: e.tensor_scalar(out=ye[ys][:, 512:1024], in0=psf(bk), scalar1=sc_ap, scalar2=None, op0=ALU.mult),
                 reads=[('ps', bk), 'tvT'], writes=[('ye', ys, 1)])

    def scatterE(e_, q):
        s_, hh = q // 2, q % 2
        ys = q % 2
        P.op('pool', lambda e: e.indirect_dma_start(
            out=ACC, out_offset=bass.IndirectOffsetOnAxis(ap=idxT[:, hh, s_ * 16 + e_: s_ * 16 + e_ + 1], axis=0),
            in_=ye[ys][:], in_offset=None, compute_op=ALU.add),
            reads=[('ye', ys, 0), ('ye', ys, 1), 'idxT'], writes=[('ACCs', s_)], dma='sc%d' % ys)

    gather(0)
    for e_ in range(NE):
        sl = e_ % 2
        if e_ + 1 < NE:
            gather(e_ + 1)
        for kp in range(4):
            trgE(sl, kp)
        for f in range(8):
            hidE(sl, f)
        for q in range(4):
            downE(e_, sl, q, 0)
            downE(e_, sl, q, 1)
            scatterE(e_, q)
        if e_ + 2 < NE:
            load_expert(e_ + 2)
        if e_ == NE - 2:
            load_w(Wpg, w_pg, 'Wpg', 8)
            load_w(Wple, w_ple, 'Wple', 2)
    P.barrier()
    if stop_after == 'E':
        return finish(nc, P, stack, dbg_outs)

    fb = Bump(nc, base, 8 * K, 186 * K)
    pool_pow[0] = True
    pgb = fb.take("pgb", [128, D], F32)
    fgb = fb.take("fgb", [128, D], F32)
    NS3 = 6
    NPF = 5
    ptf = [fb.take("ptf%d" % i, [128, 256], F32) for i in range(NS3)]
    x2 = [fb.take("x2_%d" % i, [128, D], F32) for i in range(NS3)]
    h3 = [fb.take("h3_%d" % i, [128, D], BF16) for i in range(NS3)]
    h3T = [fb.take("h3T%d" % i, [128, D], BF16) for i in range(NS3)]
    pbf = [fb.take("pbf%d" % i, [128, 256], BF16) for i in range(NS3)]
    pT = [fb.take("pT%d" % i, [128, 256], BF16) for i in range(NS3)]
    gt = [fb.take("gt%d" % i, [128, D], F32) for i in range(NS3)]
    e1 = [fb.take("e1_%d" % i, [128, D], F32) for i in range(NS3)]
    x3 = [fb.take("x3_%d" % i, [128, D], F32) for i in range(NS3)]
    ot = [fb.take("ot%d" % i, [128, D], F32) for i in range(NS3)]

    def gload(gi, gt_):
        P.op('sp', lambda e: e.dma_start(out=gt_[:], in_=gains[gi, :].partition_broadcast(128)), writes=[('gb', gi)], dma='su_gb%d' % gi)
    def fold_g3(k):
        P.op('dve', lambda e: e.tensor_scalar(out=Wpg[:, k, :], in0=Wpg[:, k, :], scalar1=cw(C_G3 + k), scalar2=None, op0=ALU.mult),
             reads=['Wpg', 'colpack'], writes=[('Wpgk', k)])
    for k in range(8):
        fold_g3(k)
    gload(3, pgb)
    gload(4, fgb)
    NTT = TOK // 128
    fstate = {}

    def f_load(i):
        slot = i % NS3
        rows = slice(i * 128, (i + 1) * 128)
        P.op('sp', lambda e: e.dma_start(out=x2[slot][:], in_=X1[rows, :]), writes=[('x2', slot)], dma='fx1_%d' % slot)
        P.op('pool', lambda e: e.dma_start(out=x2[slot][:], in_=ACC[rows, :], accum_op=ALU.add), reads=[('x2', slot)], writes=[('x2', slot)],
             dma='fac_%d' % slot)
        P.op('sp', lambda e: e.dma_start(out=ptf[slot][:], in_=p_in[rows, :]), writes=[('ptf', slot)], dma='fpt_%d' % slot)

    def f_stage1(i):
        s3, s2 = i % NS3, i % NS3
        c = rms_rstd(x2[s3][:], h3[s2][:], [('x2', s3)], [('h3', s2)])
        P.op('act', lambda e: e.activation(out=h3[s2][:], in_=x2[s3][:], func=AF.Copy, scale=st[:, c:c + 1]),
             reads=[('x2', s3), ('st', c)], writes=[('h3', s2)])
        P.op('act', lambda e: e.activation(out=pbf[s2][:], in_=ptf[s3][:], func=AF.Copy), reads=[('ptf', s3)], writes=[('pbf', s2)])

    def f_half(s2, h, ch):
        bg_, be_ = P.bank(), P.bank()

        def pg(e):
            for k in range(8):
                e.matmul(psf(bg_), lhsT=h3T[s2][:, k * 128:(k + 1) * 128], rhs=Wpg[:, k, h * 512:(h + 1) * 512], start=(k == 0), stop=False)
            return e.matmul(psf(bg_), lhsT=ones_row[0:1, :], rhs=bple_row[0:1, h * 512:(h + 1) * 512], start=False, stop=True)
        P.op('pe', pg, reads=[('h3T', s2), 'Wpg', 'ones_row', 'bple_row'] + [('Wpgk', k) for k in range(8)], writes=[('ps', bg_)])

        def pe_(e):
            r = None
            for k in range(2):
                r = e.matmul(psf(be_), lhsT=pT[s2][:, k * 128:(k + 1) * 128], rhs=Wple[:, k, h * 512:(h + 1) * 512], start=(k == 0), stop=(k == 1))
            return r
        P.op('pe', pe_, reads=[('pT', s2), 'Wple'], writes=[('ps', be_)])
        P.op('act', lambda e: e.activation(out=gt[s2][:, h * 512:(h + 1) * 512], in_=psf(bg_), func=AF.Sigmoid), reads=[('ps', bg_)], writes=[('gt', s2, h)])
        P.op('act', lambda e: e.activation(out=e1[s2][:, h * 512:(h + 1) * 512], in_=psf(be_), func=AF.Copy), reads=[('ps', be_)], writes=[('e1', s2, h)])
        return be_

    def f_stage2(i):
        s2 = i % NS3
        bt, bp = P.bank(), P.bank()

        def tr3(e):
            r = None
            for k in range(8):
                r = e.transpose(out=psb(bt)[:, k * 128:(k + 1) * 128], in_=h3[s2][:, k * 128:(k + 1) * 128], identity=ident_bf[:])
            return r
        P.op('pe', tr3, reads=[('h3', s2), 'ident_bf'], writes=[('ps', bt)])

        def tr4(e):
            r = None
            for k in range(2):
                r = e.transpose(out=psb(bp)[:, k * 128:(k + 1) * 128], in_=pbf[s2][:, k * 128:(k + 1) * 128], identity=ident_bf[:])
            return r
        P.op('pe', tr4, reads=[('pbf', s2), 'ident_bf'], writes=[('ps', bp)])
        P.op('dve', lambda e: e.tensor_copy(out=h3T[s2][:], in_=psb(bt)), reads=[('ps', bt)], writes=[('h3T', s2)])
        P.op('dve', lambda e: e.tensor_copy(out=pT[s2][:], in_=psb(bp)[:, 0:256]), reads=[('ps', bp)], writes=[('pT', s2)])
        f_half(s2, 0, None)
        f_half(s2, 1, None)

    def f_e1(s2, h, cr):
        hs = slice(h * 512, (h + 1) * 512)
        P.op('dve', lambda e: e.scalar_tensor_tensor(out=e1[s2][:, hs], in0=e1[s2][:, hs], scalar=st[:, cr:cr + 1], in1=pgb[:, hs], op0=ALU.mult, op1=ALU.mult),
             reads=[('e1', s2, h), ('st', cr), ('gb', 3)], writes=[('e1', s2, h)])

    def f_stage3(i):
        s3, s2 = i % NS3, i % NS3
        cr = rms_rstd(e1[s2][:], x3[s2][:], [('e1', s2, 0), ('e1', s2, 1)], [('x3', s2)])
        f_e1(s2, 0, cr)
        f_e1(s2, 1, cr)
        P.op('dve', lambda e: e.tensor_tensor(out=x3[s2][:], in0=gt[s2][:], in1=e1[s2][:], op=ALU.mult),
             reads=[('gt', s2, 0), ('gt', s2, 1), ('e1', s2, 0), ('e1', s2, 1)], writes=[('x3', s2)])
        P.op('dve', lambda e: e.tensor_tensor(out=x3[s2][:], in0=x3[s2][:], in1=x2[s3][:], op=ALU.add), reads=[('x3', s2), ('x2', s3)], writes=[('x3', s2)])
        c2_ = rms_rstd(x3[s2][:], ot[s2][:], [('x3', s2)], [('ot', s2)])
        P.op('dve', lambda e: e.scalar_tensor_tensor(out=ot[s2][:], in0=x3[s2][:], scalar=st[:, c2_:c2_ + 1], in1=fgb[:], op0=ALU.mult, op1=ALU.mult),
             reads=[('x3', s2), ('st', c2_), ('gb', 4)], writes=[('ot', s2)])
        P.op('sp', lambda e: e.dma_start(out=out[i * 128:(i + 1) * 128, :], in_=ot[s2][:]), reads=[('ot', s2)], writes=[('out', i)], dma='ost%d' % s2)

    for i in range(min(NPF, NTT)):
        f_load(i)
    for i in range(NTT):
        if i + NPF < NTT:
            f_load(i + NPF)
        f_stage1(i)
        f_stage2(i)
        f_stage3(i)
    return finish(nc, P, stack, dbg_outs)


def finish(nc, P, stack, dbg_outs):
    streams = P.finalize()
    print("[sched] ops=%d est_us=%.1f" % (len(P.ops), P.est_us))
    with stack:
        with nc.Block() as block:
            @block.tensor
            def _(e):
                for f in streams['pe']:
                    f(e)

            @block.scalar
            def _(e):
                for f in streams['act']:
                    f(e)

            @block.vector
            def _(e):
                for f in streams['dve']:
                    f(e)

            @block.gpsimd
            def _(e):
                for f in streams['pool']:
                    f(e)

            @block.sync
            def _(e):
                for f in streams['sp']:
                    f(e)
    return nc, dbg_outs


def make_in_maps(inputs, ncores=8):
    g = lambda k: np.asarray(inputs[k], dtype=np.float32)
    x = g("x")
    p = g("p")[0]
    conv_w = g("conv_w")[0]
    colpack = np.zeros((128, 168), np.float32)
    colpack[:, 0:16] = g("b_gate")[0].reshape(16, 128).T
    colpack[:, 16:140] = conv_w.T.reshape(4, 128, 31).transpose(1, 0, 2).reshape(128, 124)
    colpack[:, 140:144] = g("conv_b")[0].reshape(4, 128).T
    colpack[:, 144:148] = g("conv_ln_g")[0].reshape(4, 128).T
    colpack[:, 148:152] = g("conv_ln_b")[0].reshape(4, 128).T
    colpack[:, 152:160] = g("pool_scale")[0].reshape(8, 128).T
    colpack[:, 160:168] = g("norm3_g")[0].reshape(8, 128).T
    gains = np.stack([g("norm1_g")[0], g("norm2_g")[0], g("norm3_g")[0], g("ple_norm_g")[0], g("final_g")], axis=0)
    shared = {
        "w_in": np.ascontiguousarray(g("w_in")[0]),
        "w_conv_out": np.ascontiguousarray(g("w_conv_out")[0]),
        "w_pool": np.ascontiguousarray(g("w_pool")[0]),
        "w_out": np.ascontiguousarray(g("w_out")[0]),
        "w_router": np.ascontiguousarray(g("w_router")[0]),
        "w_exp_gate": np.ascontiguousarray(g("w_exp_gate")[0]),
        "w_exp_up": np.ascontiguousarray(g("w_exp_up")[0]),
        "w_exp_down": np.ascontiguousarray(g("w_exp_down")[0]),
        "w_ple_gate": np.ascontiguousarray(g("w_ple_gate")[0]),
        "w_ple": np.ascontiguousarray(g("w_ple")[0]),
        "b_ple_gate": np.ascontiguousarray(g("b_ple_gate")[0].reshape(1, D)),
        "gains": np.ascontiguousarray(gains),
        "colpack": colpack,
        "ident": np.eye(128, dtype=np.float32),
        "zeros": np.zeros((512, D), np.float32),
    }
    maps = []
    for c in range(ncores):
        m = dict(shared)
        m["x"] = np.ascontiguousarray(x[2 * c:2 * c + 2].reshape(TOK, D))
        m["p"] = np.ascontiguousarray(p[2 * c:2 * c + 2].reshape(TOK, 256))
        maps.append(m)
    return maps


_CACHE = {}


def kernel(**inputs):
    if "nc" not in _CACHE:
        _CACHE["nc"] = build()[0]
    nc = _CACHE["nc"]
    maps = make_in_maps(inputs)
    res = run_bass_kernel_spmd(nc, maps, core_ids=list(range(8)))
    outs = [np.asarray(r["out"], dtype=np.float32).reshape(2, S, D) for r in res.results]
    return np.concatenate(outs, axis=0)
```

```python
import numpy as np
from contextlib import ExitStack
import concourse.bass as bass
import concourse.mybir as mybir
from concourse.bass_utils import run_bass_kernel_spmd

F32 = mybir.dt.float32
BF16 = mybir.dt.bfloat16
I32 = mybir.dt.int32
U32 = mybir.dt.uint32
AF = mybir.ActivationFunctionType
ALU = mybir.AluOpType
AX = mybir.AxisListType

S = 2048
D = 1024
NSEQ = 2
TOK = NSEQ * S
NE = 16
CAP = 256
EPS = 1e-6
K = 1024
DTSIZE = {F32: 4, BF16: 2, I32: 4, U32: 4}
COMPUTE = ('pe', 'act', 'dve', 'pool')


class CostProbe:
    def __init__(self, eng):
        self.eng = eng
        self.cost = 0.0
        self.lat = 0.0
        self.cls = None

    def __getattr__(self, name):
        def f(*args, **kw):
            out = kw.get('out', args[0] if args else None)
            shape = tuple(out.shape)
            free = 1
            for d in shape[1:]:
                free *= d
            if name == 'matmul':
                lhsT = kw.get('lhsT')
                mult = 4.0 if (lhsT is not None and lhsT.dtype == F32) else 1.0
                self.cost += (0.03 + free / 1950.0) * mult
            elif name == 'transpose':
                self.cost += 0.07
            elif name in ('dma_start', 'indirect_dma_start'):
                nbytes = free * shape[0] * DTSIZE.get(out.dtype, 4)
                self.cost += 0.1 if self.eng == 'sp' else 1.0
                self.lat = max(self.lat, 2.0 + nbytes / 150e3)
            else:
                per = 0.0023 if self.eng == 'pool' else 0.00105
                self.cost += 0.2 + free * per
                if name == 'activation':
                    fn = kw.get('func', args[2] if len(args) > 2 else None)
                    if fn in (AF.Sigmoid, AF.Silu):
                        self.cls = 'sig'
                    elif fn == AF.Sqrt:
                        self.cls = 'sqrt'
                    elif fn in (AF.Exp, AF.Ln):
                        self.cls = 'exp'
            return self
        return f


class Op:
    __slots__ = ('idx', 'eng', 'emit', 'dma', 'ndma', 'deps', 'cost', 'lat', 'rec', 'seg', 'cls')


class Prog:
    ENG = ['pe', 'act', 'dve', 'pool', 'sp']
    WINDOW = 48
    LAT = 0.2
    TBL = 1.3
    cur_cls = None
    PERSIST = ('w16',)
    BACKGROUND = ('cv',)

    def __init__(self, nc, stack):
        self.nc = nc
        self.stack = stack
        self.ops = []
        self.segs = [[]]
        self.res = {}
        self.nbank = 0
        self.ncol = 0
        self.sems = {}
        for e in COMPUTE:
            self._sem(e)

    def _sem(self, name):
        if name not in self.sems:
            self.sems[name] = self.stack.enter_context(self.nc.semaphore("s_" + name))
        return self.sems[name]

    def bank(self):
        b = self.nbank % 8
        self.nbank += 1
        return b

    def col(self):
        c = self.ncol % 256
        self.ncol += 1
        return c

    def op(self, eng, emit, reads=(), writes=(), dma=None, ndma=1, after=()):
        o = Op()
        o.idx = len(self.ops)
        o.eng, o.emit, o.dma, o.ndma = eng, emit, dma, ndma
        deps = set()
        for r in reads:
            st = self.res.get(r)
            if st and st[0] is not None:
                deps.add(st[0])
        for w in writes:
            st = self.res.get(w)
            if st:
                if st[0] is not None:
                    deps.add(st[0])
                deps |= st[1]
        for r in after:
            st = self.res.get(r)
            if st and st[0] is not None:
                deps.add(st[0])
        o.deps = deps
        pr = CostProbe(eng)
        emit(pr)
        o.cost, o.lat, o.cls = pr.cost, pr.lat, pr.cls
        if dma is not None:
            self._sem(dma)
        for r in reads:
            st = self.res.setdefault(r, [None, set()])
            st[1].add(o.idx)
        for w in writes:
            self.res[w] = [o.idx, set()]
        o.seg = len(self.segs) - 1
        self.ops.append(o)
        self.segs[-1].append(o)

    def barrier(self):
        self.segs.append([])
        self.res = {k: v for k, v in self.res.items() if isinstance(k, tuple) and k[0] in self.PERSIST}

    def _schedule_seg(self, seg):
        ENG = self.ENG
        rem = {e: [o for o in seg if o.eng == e] for e in ENG}
        pos = {e: 0 for e in ENG}
        done = {}
        eng_free = {e: 0.0 for e in ENG}
        order = {e: [] for e in ENG}
        sched = set()
        segidx = set(o.idx for o in seg)
        n_left = len(seg)
        while n_left:
            best = None
            for e in ENG:
                lst = rem[e]
                p = pos[e]
                while p < len(lst) and lst[p].idx in sched:
                    p += 1
                pos[e] = p
                seen_dma = False
                cnt = 0
                q = p
                while q < len(lst) and cnt < self.WINDOW:
                    o = lst[q]
                    q += 1
                    if o.idx in sched:
                        continue
                    cnt += 1
                    if o.dma is not None:
                        if seen_dma:
                            continue
                        seen_dma = True
                    t = eng_free[e]
                    ok = True
                    for d in o.deps:
                        if d in segidx:
                            fd = done.get(d)
                            if fd is None:
                                ok = False
                                break
                            if fd + self.LAT > t:
                                t = fd + self.LAT
                    if not ok:
                        continue
                    pen = self.TBL if (o.cls is not None and o.cls != self.cur_cls) else 0.0
                    key = (t + pen, o.idx)
                    if best is None or key < best[0]:
                        best = (key, o, pen)
                    if t + pen <= eng_free[e]:
                        break
            assert best is not None, "scheduler deadlock"
            (t, _), o, pen = best
            if o.cls is not None:
                self.cur_cls = o.cls
            sched.add(o.idx)
            eng_free[o.eng] = t + o.cost
            done[o.idx] = t + o.cost + o.lat
            order[o.eng].append(o)
            n_left -= 1
        return order, max(eng_free.values())

    def finalize(self):
        sems = self.sems
        streams = {e: [] for e in self.ENG}
        total = {s: 0 for s in sems}
        known = {e: {} for e in self.ENG}
        est = 0.0
        orders, snaps = [], []
        for seg in self.segs:
            order, t_end = self._schedule_seg(seg)
            est += t_end
            for e in self.ENG:
                for o in order[e]:
                    if o.dma is None:
                        total[e] += 1
                        o.rec = (e, total[e])
                    else:
                        total[o.dma] += 16 * o.ndma
                        o.rec = (o.dma, total[o.dma])
            orders.append(order)
            snaps.append(dict(total))
        final = dict(total)
        for si, order in enumerate(orders):
            for e in self.ENG:
                for o in order[e]:
                    waits = []
                    need = {}
                    for d in o.deps:
                        s_, v_ = self.ops[d].rec
                        if s_.startswith(self.BACKGROUND):
                            v_ = final[s_]
                        if need.get(s_, 0) < v_:
                            need[s_] = v_
                    for s_, v_ in need.items():
                        if s_ == 'pe' and e == 'pe':
                            continue
                        if known[e].get(s_, 0) < v_:
                            waits.append((s_, v_))
                            known[e][s_] = v_
                    sem = sems[o.rec[0]]

                    def run(eh, waits=waits, o=o, sem=sem):
                        for s_, v_ in waits:
                            eh.wait_ge(sems[s_], v_)
                        r = o.emit(eh)
                        if isinstance(r, (list, tuple)):
                            assert o.dma is not None and len(r) == o.ndma
                            for x in r:
                                x.then_inc(sem, 16)
                        else:
                            assert o.dma is None or o.ndma == 1
                            r.then_inc(sem, 16 if o.dma is not None else 1)
                    streams[e].append(run)
            last = si == len(orders) - 1
            snap = {s_: v_ for s_, v_ in snaps[si].items() if last or not s_.startswith(self.BACKGROUND)}
            for e in self.ENG:
                waits = [(s_, v_) for s_, v_ in snap.items() if v_ > 0 and known[e].get(s_, 0) < v_]
                for s_, v_ in waits:
                    known[e][s_] = v_

                def runb(eh, waits=waits):
                    for s_, v_ in waits:
                        eh.wait_ge(sems[s_], v_)
                streams[e].append(runb)
        self.est_us = est
        return streams


class Bump:
    def __init__(self, nc, base, start, end):
        self.nc, self.base, self.cur, self.end = nc, base, start, end
        self.n = 0

    def take(self, name, shape, dtype):
        nbytes = int(np.prod(shape[1:])) * DTSIZE[dtype]
        nbytes = (nbytes + 31) // 32 * 32
        assert self.cur + nbytes <= self.end, (name, self.cur, nbytes, self.end)
        h = self.nc.alloc_sbuf_tensor_at(name, list(shape), dtype, offset=self.base + self.cur)
        self.cur += nbytes
        return h


def build(debug=False, stop_after=None):
    nc = bass.Bass("TRN2", target_bir_lowering=False)
    dbg_outs = []

    def dram(name, shape, dt, kind):
        return nc.dram_tensor(name, list(shape), dt, kind=kind).ap()

    x = dram("x", [TOK, D], F32, "ExternalInput")
    p_in = dram("p", [TOK, 256], F32, "ExternalInput")
    w_in = dram("w_in", [D, 3584], F32, "ExternalInput")
    w_co = dram("w_conv_out", [512, D], F32, "ExternalInput")
    w_pool = dram("w_pool", [4, 128, 256], F32, "ExternalInput")
    w_out = dram("w_out", [D, D], F32, "ExternalInput")
    w_router = dram("w_router", [D, NE], F32, "ExternalInput")
    w_eg = dram("w_exp_gate", [NE, D, D], F32, "ExternalInput")
    w_eu = dram("w_exp_up", [NE, D, D], F32, "ExternalInput")
    w_ed = dram("w_exp_down", [NE, D, D], F32, "ExternalInput")
    w_pg = dram("w_ple_gate", [D, D], F32, "ExternalInput")
    w_ple = dram("w_ple", [256, D], F32, "ExternalInput")
    b_ple = dram("b_ple_gate", [1, D], F32, "ExternalInput")
    gains = dram("gains", [5, D], F32, "ExternalInput")
    colpack_d = dram("colpack", [128, 168], F32, "ExternalInput")
    ident_d = dram("ident", [128, 128], F32, "ExternalInput")
    zeros_d = dram("zeros", [512, D], F32, "ExternalInput")
    out = dram("out", [TOK, D], F32, "ExternalOutput")
    X1 = dram("x1_scr", [TOK, D], F32, "Internal")
    H2 = dram("h2_scr", [TOK, D], BF16, "Internal")
    ACC = dram("acc_scr", [TOK, D], F32, "Internal")
    W16 = [dram("w16_%d" % m, [NE, D, D], BF16, "Internal") for m in range(3)]

    ARENA = 206 * K
    NXT = 3
    NHB = 6
    arena = nc.alloc_sbuf_tensor("arena", [128, ARENA // 4], F32)
    base = nc.lookup_mloc(arena).addr
    ps = nc.alloc_psum_tensor("ps", [128, 8, 512], F32)

    def psf(b):
        return ps[:, b, :]

    def psb(b):
        return ps[:, b, :].bitcast(BF16)

    stack = ExitStack()
    P = Prog(nc, stack)

    pers = Bump(nc, base, 0, 8 * K)
    ident_bf = pers.take("ident_bf", [128, 128], BF16)
    ident_f = pers.take("ident_f", [128, 128], F32)
    onesm = pers.take("onesm", [128, 128], F32)
    ones_row = pers.take("ones_row", [1, 128], BF16)
    bple_row = pers.take("bple_row", [1, D], BF16)
    colpack = pers.take("colpack_s", [128, 168], F32)
    wr = pers.take("wr", [128, 8, NE], BF16)
    aff_all = pers.take("aff_all", [128, 16, 32], F32)
    idxT = pers.take("idxT", [128, 2, 32], I32)
    tvT = pers.take("tvT", [128, 2, 32], F32)
    epsT = pers.take("eps", [128, 1], F32)
    mhalf = pers.take("mhalf", [128, 1], F32)
    st = pers.take("st", [128, 256], F32)
    C_BG, C_CW, C_CB, C_LG, C_LB, C_PS, C_G3 = 0, 16, 140, 144, 148, 152, 160

    def dump(name, ap, reads, eng='sp'):
        if not debug:
            return
        t = dram("dbg_" + name, list(ap.shape), ap.dtype, "ExternalOutput")
        dbg_outs.append("dbg_" + name)
        P.op('sp', lambda e: e.dma_start(out=t, in_=ap), reads=reads, dma='dbg_' + name)

    P.op('sp', lambda e: e.dma_start(out=ident_f[:], in_=ident_d), writes=['ident_f'], dma='su_ident_f')
    P.op('sp', lambda e: e.dma_start(out=colpack[:], in_=colpack_d), writes=['colpack'], dma='su_colpack')
    P.op('pool', lambda e: e.dma_start(out=ident_bf[:], in_=ident_d), writes=['ident_bf'], dma='su_ident_bf')
    P.op('pool', lambda e: e.dma_start(out=wr[:], in_=w_router.rearrange("(j p) e -> p j e", p=128)),
         writes=['wr'], dma='su_wr')
    P.op('pool', lambda e: e.dma_start(out=bple_row[:], in_=b_ple), writes=['bple_row'], dma='su_bple')
    P.op('dve', lambda e: e.memset(onesm[:], 1.0 / 512), writes=['onesm'])
    P.op('dve', lambda e: e.memset(ones_row[:], 1.0), writes=['ones_row'])
    P.op('dve', lambda e: e.memset(epsT[:], EPS), writes=['eps'])
    P.op('dve', lambda e: e.memset(mhalf[:], -0.5), writes=['mhalf'])

    def zero_acc():
        for j in range(TOK // 512):
            P.op('sp', lambda e, j=j: e.dma_start(out=ACC[j * 512:(j + 1) * 512, :], in_=zeros_d), writes=[('ACC', j)], dma='accz')

    gb = Bump(nc, base, 8 * K, 20 * K)
    g1b = gb.take("g1b", [128, D], F32)
    g2b = gb.take("g2b", [128, D], F32)
    xt_extra = gb.take("xtA2", [128, D], F32)
    mm = Bump(nc, base, 20 * K, 143 * K)
    hT = mm.take("hT", [128, 8, S], BF16)
    dd = mm.take("dd", [128, 4, S], BF16)
    WA = mm.take("WA", [128, 8, 2048], BF16)
    Wco = mm.take("Wco", [128, 4, D], BF16)
    Wpool = mm.take("Wpool", [128, 4, 256], BF16)
    Wout = mm.take("Wout", [128, 8, D], BF16)
    VW = 2080
    vpad = mm.take("vpad", [128, 4, VW], BF16)
    sa = Bump(nc, base, 143 * K, 206 * K)
    UW = 2064
    upad = sa.take("upad", [128, 4, UW], F32)
    XT = [sa.take("xtA%d" % i, [128, D], F32) for i in range(2)] + [xt_extra]
    hb = sa.take("hb", [128, NHB, D], BF16)
    sgt = [sa.take("sgt%d" % i, [128, 512], F32) for i in range(2)]
    tA = sa.take("tA", [128, 528], F32)
    tB = sa.take("tB", [128, 528], F32)
    sb = Bump(nc, base, 143 * K, 206 * K)
    c_act = sb.take("c_act", [128, 4, S], BF16)
    conv_blk = sb.take("conv_blk", [128, 4, 512], F32)
    sq = sb.take("sq", [128, 4, 512], F32)
    sb.cur = 143 * K + 33024
    dg = [sb.take("dg%d" % i, [128, 31, 128], BF16) for i in range(2)]
    m2t = sb.take("m2t", [128, 512], F32)
    vart = sb.take("vart", [128, 512], F32)
    rstdt = sb.take("rstdt", [128, 512], F32)
    nbt = sb.take("nbt", [128, 512], F32)
    tt = [sb.take("tt%d" % i, [128, 512], F32) for i in range(2)]
    sc = Bump(nc, base, 143 * K + 16 * K, 206 * K)
    sgc = [sc.take("sgc%d" % i, [128, 512], F32) for i in range(2)]
    sgp = [sc.take("sgp%d" % i, [128, 512], F32) for i in range(2)]
    m1t = [sc.take("m1t%d" % i, [128, 512], F32) for i in range(2)]
    m2c = [sc.take("m2c%d" % i, [128, 512], F32) for i in range(2)]
    mgs = [sc.take("mg%d" % i, [128, 8, 512], BF16) for i in range(2)]
    XTC = [sc.take("xtC%d" % i, [128, D], F32) for i in range(2)]
    x1t = [sc.take("x1t0", [128, D], F32)]
    sv = Bump(nc, base, mm.cur - 4 * VW * 2 - 0, mm.cur)
    x1t.append(sv.take("x1t1", [128, D], F32))
    XTC.append(sv.take("xtC2", [128, D], F32))
    XTC.append(xt_extra)
    h2t = [sv.take("h2t%d" % i, [128, D], BF16) for i in range(2)]
    h2T = [sv.take("h2T%d" % i, [128, D], BF16) for i in range(2)]
    ext = sv.take("ext", [128, 32], F32)

    P.op('sp', lambda e: e.dma_start(out=g1b[:], in_=gains[0, :].partition_broadcast(128)), writes=['g1b'], dma='su_g1b')
    P.op('sp', lambda e: e.dma_start(out=g2b[:], in_=gains[1, :].partition_broadcast(128)), writes=['g2b'], dma='su_g2b')

    def load_w(dst, src, key, nk, eng='pool'):
        def emit(e):
            return [e.dma_start(out=dst[:, k, :], in_=src[k * 128:(k + 1) * 128, :]) for k in range(nk)]
        P.op(eng, emit, writes=[key], dma='w_' + key if isinstance(key, str) else 'w_' + "_".join(map(str, key)), ndma=nk)

    load_w(Wco, w_co, 'Wco', 4)
    P.op('pool', lambda e: [e.dma_start(out=Wpool[:, g, :], in_=w_pool[g, :, :]) for g in range(4)],
         writes=['Wpool'], dma='w_Wpool', ndma=4)
    load_w(Wout, w_out, 'Wout', 8)

    cw = lambda c: colpack[:, c:c + 1]

    pool_pow = [False]

    def rms_rstd(src_ap, junk_ap, reads, jkeys):
        c0, c1, c2 = P.col(), P.col(), P.col()
        P.op('dve', lambda e: e.memset(st[:, c0:c0 + 1], 0.0), writes=[('st', c0)])
        P.op('act', lambda e: e.activation(out=junk_ap, in_=src_ap, func=AF.Square, accum_out=st[:, c0:c0 + 1]),
             reads=list(reads) + [('st', c0)], writes=[('st', c0)] + list(jkeys))
        if pool_pow[0]:
            P.op('pool', lambda e: e.tensor_scalar(out=st[:, c1:c1 + 1], in0=st[:, c0:c0 + 1], scalar1=1.0 / D, scalar2=EPS, op0=ALU.mult, op1=ALU.add),
                 reads=[('st', c0)], writes=[('st', c1)])
            P.op('pool', lambda e: e.tensor_tensor(out=st[:, c2:c2 + 1], in0=st[:, c1:c1 + 1], in1=mhalf[:, 0:1], op=ALU.pow),
                 reads=[('st', c1), 'mhalf'], writes=[('st', c2)])
            return c2
        P.op('act', lambda e: e.activation(out=st[:, c1:c1 + 1], in_=st[:, c0:c0 + 1], func=AF.Sqrt,
                                           bias=epsT[:, 0:1], scale=1.0 / D),
             reads=[('st', c0), 'eps'], writes=[('st', c1)])
        P.op('dve', lambda e: e.reciprocal(out=st[:, c2:c2 + 1], in_=st[:, c1:c1 + 1]),
             reads=[('st', c1)], writes=[('st', c2)])
        return c2

    cntA = [0]
    cntC = [0]
    hbmap = {}
    GA_early = [('hT', b_, jp_) for b_ in range(4) for jp_ in range(4)] + [('hb', h_) for h_ in range(NHB)]
    GA_all = GA_early + [('v', b_, i_) for b_ in range(4) for i_ in range(4)] + [('d', b_, i_) for b_ in range(4) for i_ in range(4)]
    GC = [('ca', 3, i_) for i_ in range(4)]
    VKEYS = [('v', b_, i_) for b_ in range(4) for i_ in range(4)] + [('vz', i_, z_) for i_ in range(4) for z_ in range(2)]
    W32 = (w_eg, w_eu, w_ed)
    conv_list = [(e_, m) for e_ in range(NE) for m in range(3)]
    conv_pos = [0]

    def convert_weights(n, dep=None):
        for _ in range(n):
            if conv_pos[0] >= len(conv_list):
                return
            e_, m = conv_list[conv_pos[0]]
            conv_pos[0] += 1
            P.op('pool', lambda e, e_=e_, m=m: [e.dma_start(out=W16[m][e_, hh * 512:(hh + 1) * 512, :], in_=W32[m][e_, hh * 512:(hh + 1) * 512, :], max_dma_last_dim=2048) for hh in range(2)],
                 after=([dep] if dep is not None else []), writes=[('w16', e_, m)], dma='cv%d' % (conv_pos[0] % 4), ndma=2)

    def tileA(s, b, tl):
        r0 = s * S
        i = 4 * b + tl
        slot = cntA[0] % NXT
        hs = cntA[0] % NHB
        cntA[0] += 1
        hbmap[(s, b, tl)] = hs
        xt = XT[slot]
        src = x[r0 + i * 128: r0 + (i + 1) * 128, :]
        P.op('sp', lambda e: e.dma_start(out=xt[:], in_=src), writes=[('xtA', slot)], dma='xtA%d' % slot, after=gcend(s))
        c = rms_rstd(xt[:], hb[:, hs, :], [('xtA', slot)], [('hb', hs)])
        P.op('dve', lambda e: e.scalar_tensor_tensor(
            out=hb[:, hs, :], in0=xt[:], scalar=st[:, c:c + 1], in1=g1b[:], op0=ALU.mult, op1=ALU.mult),
            reads=[('xtA', slot), ('st', c), 'g1b'], writes=[('hb', hs)])

    def transA(s, b, jp):
        bk = P.bank()
        hss = [hbmap[(s, b, tl)] for tl in range(4)]

        def tr(e):
            r = None
            for jj in range(2):
                for tl in range(4):
                    r = e.transpose(out=psb(bk)[:, (jj * 4 + tl) * 128:(jj * 4 + tl + 1) * 128],
                                    in_=hb[:, hss[tl], (2 * jp + jj) * 128:(2 * jp + jj + 1) * 128], identity=ident_bf[:])
            return r
        P.op('pe', tr, reads=[('hb', t_) for t_ in hss] + ['ident_bf'], writes=[('ps', bk)])
        src = psb(bk).rearrange("p (j t) -> p j t", j=2)
        dst = hT[:, 2 * jp:2 * jp + 2, b * 512:(b + 1) * 512]
        if jp % 2 == 0:
            P.op('act', lambda e: e.activation(out=dst, in_=src, func=AF.Copy), reads=[('ps', bk)], writes=[('hT', b, jp)])
        else:
            P.op('dve', lambda e: e.tensor_copy(out=dst, in_=src), reads=[('ps', bk)], writes=[('hT', b, jp)])

    def mm_hT(bk, W, c0, b):
        def emit(e):
            r = None
            for k in range(8):
                r = e.matmul(psf(bk), lhsT=W[:, k, c0:c0 + 128], rhs=hT[:, k, b * 512:(b + 1) * 512], start=(k == 0), stop=(k == 7))
            return r
        return emit

    def pairA(b, i):
        hTr = [('hT', b, jp) for jp in range(4)]
        ba, bb_ = P.bank(), P.bank()
        P.op('pe', mm_hT(ba, WA, i * 128, b), reads=hTr + ['WA'], writes=[('ps', ba)])
        P.op('pe', mm_hT(bb_, WA, 512 + i * 128, b), reads=hTr + ['WA'], writes=[('ps', bb_)])
        sl = (4 * b + i) % 2
        P.op('act', lambda e: e.activation(out=sgt[sl][:], in_=psf(bb_), func=AF.Sigmoid), reads=[('ps', bb_)], writes=[('sgt', sl)])
        P.op('dve', lambda e: e.tensor_tensor(out=vpad[:, i, 15 + b * 512: 15 + (b + 1) * 512], in0=psf(ba), in1=sgt[sl][:], op=ALU.mult),
             reads=[('ps', ba), ('sgt', sl)], writes=[('v', b, i)])

    def uA(b, i):
        hTr = [('hT', b, jp) for jp in range(4)]
        bu = P.bank()
        P.op('pe', mm_hT(bu, WA, 1024 + i * 128, b), reads=hTr + ['WA'], writes=[('ps', bu)])
        dst = upad[:, i, 8 + b * 512: 8 + (b + 1) * 512]
        if i % 2 == 0:
            P.op('act', lambda e: e.activation(out=dst, in_=psf(bu), func=AF.Copy), reads=[('ps', bu)], writes=[('u', b, i)])
        else:
            P.op('dve', lambda e: e.tensor_copy(out=dst, in_=psf(bu)), reads=[('ps', bu)], writes=[('u', b, i)])

    def u_ap(i, lo, n):
        return upad[:, i, 8 + lo: 8 + lo + n]

    def dve_add(o, a, b_, reads, writes):
        P.op('dve', lambda e: e.tensor_tensor(out=o, in0=a, in1=b_, op=ALU.add), reads=reads, writes=writes)

    def pool_fin(bb, i, wbuf, wkey):
        T0 = bb * 512
        h = 1 << i
        ur = [('u', b2, i) for b2 in range(max(0, bb - 1), min(3, bb + 1) + 1)] + [('uz', i, 0), ('uz', i, 1)]
        P.op('dve', lambda e: e.scalar_tensor_tensor(out=dd[:, i, T0:T0 + 512], in0=wbuf[:, 0:512], scalar=1.0 / (2 * h),
                                                     in1=u_ap(i, T0, 512), op0=ALU.mult, op1=ALU.subtract),
             reads=[wkey] + ur, writes=[('d', bb, i)])
        cols = []
        if bb == 0:
            cols += [(t, t + h) for t in range(0, h)]
        if bb == 3:
            cols += [(t, S - t + h) for t in range(S - h + 1, S)]
        if cols:
            def emit(e):
                r = None
                for t, cnt in cols:
                    r = e.scalar_tensor_tensor(out=dd[:, i, t:t + 1], in0=wbuf[:, t - T0:t - T0 + 1], scalar=1.0 / cnt,
                                               in1=u_ap(i, t, 1), op0=ALU.mult, op1=ALU.subtract)
                return r
            P.op('dve', emit, reads=[wkey] + ur, writes=[('d', bb, i)])

    def pool_block(bb):
        T0 = bb * 512
        ur = lambda i: [('u', b2, i) for b2 in range(max(0, bb - 1), min(3, bb + 1) + 1)] + [('uz', i, 0), ('uz', i, 1)]
        dve_add(tA[:, 0:512], u_ap(0, T0 - 1, 512), u_ap(0, T0, 512), ur(0), ['tA'])
        pool_fin(bb, 0, tA, 'tA')
        dve_add(tB[:, 0:514], u_ap(1, T0 - 2, 514), u_ap(1, T0 - 1, 514), ur(1), ['tB'])
        dve_add(tA[:, 0:512], tB[:, 0:512], tB[:, 2:514], ['tB'], ['tA'])
        pool_fin(bb, 1, tA, 'tA')
        dve_add(tA[:, 0:518], u_ap(2, T0 - 4, 518), u_ap(2, T0 - 3, 518), ur(2), ['tA'])
        dve_add(tB[:, 0:516], tA[:, 0:516], tA[:, 2:518], ['tA'], ['tB'])
        dve_add(tA[:, 0:512], tB[:, 0:512], tB[:, 4:516], ['tB'], ['tA'])
        pool_fin(bb, 2, tA, 'tA')
        dve_add(tA[:, 0:526], u_ap(3, T0 - 8, 526), u_ap(3, T0 - 7, 526), ur(3), ['tA'])
        dve_add(tB[:, 0:524], tA[:, 0:524], tA[:, 2:526], ['tA'], ['tB'])
        dve_add(tA[:, 0:520], tB[:, 0:520], tB[:, 4:524], ['tB'], ['tA'])
        dve_add(tB[:, 0:512], tA[:, 0:512], tA[:, 8:520], ['tA'], ['tB'])
        pool_fin(bb, 3, tB, 'tB')

    def gcend(s):
        if s == 0:
            return []
        return [(k_, s - 1, i_) for k_ in ('aff', 'X1', 'H2') for i_ in range(16)]

    def zpadA(s, i):
        g_ = gcend(s)
        P.op('dve', lambda e: e.memset(vpad[:, i, 0:15], 0.0), writes=[('vz', i, 0)], after=g_)
        P.op('dve', lambda e: e.memset(vpad[:, i, 15 + S:VW], 0.0), writes=[('vz', i, 1)], after=g_)
        P.op('dve', lambda e: e.memset(upad[:, i, 0:8], 0.0), writes=[('uz', i, 0)], after=g_)
        P.op('dve', lambda e: e.memset(upad[:, i, 8 + S:UW], 0.0), writes=[('uz', i, 1)], after=g_)

    def convB(b, i):
        sl = (4 * b + i) % 2
        P.op('dve', lambda e: e.tensor_tensor(
            out=dg[sl][:], in0=ident_bf[:].unsqueeze(1).broadcast_to([128, 31, 128]),
            in1=colpack[:, C_CW + i * 31: C_CW + (i + 1) * 31].unsqueeze(2).broadcast_to([128, 31, 128]),
            op=ALU.mult), reads=['ident_bf', 'colpack'], writes=[('dg', sl)], after=GA_early)
        bk = P.bank()

        def conv(e):
            r = None
            for k in range(31):
                r = e.matmul(psf(bk), lhsT=dg[sl][:, k, :], rhs=vpad[:, i, b * 512 + k: b * 512 + k + 512],
                             start=(k == 0), stop=(k == 30))
            return r
        P.op('pe', conv, reads=[('dg', sl)] + VKEYS, writes=[('ps', bk)])
        P.op('dve', lambda e: e.tensor_scalar(out=conv_blk[:, i, :], in0=psf(bk), scalar1=cw(C_CB + i), scalar2=None, op0=ALU.add),
             reads=[('ps', bk), 'colpack'], writes=[('cb', i)], after=GA_all)
        P.op('act', lambda e: e.activation(out=sq[:, i, :], in_=conv_blk[:, i, :], func=AF.Square),
             reads=[('cb', i)], writes=[('sq', i)], after=GA_all)

    def statB(bk, src, keys):
        def emit(e):
            r = None
            for i in range(4):
                r = e.matmul(psf(bk), lhsT=onesm[:], rhs=src[:, i, :], start=(i == 0), stop=(i == 3))
            return r
        P.op('pe', emit, reads=keys + ['onesm'], writes=[('ps', bk)])

    def lnB(b):
        bm, bq = P.bank(), P.bank()
        statB(bm, conv_blk, [('cb', i) for i in range(4)])
        statB(bq, sq, [('sq', i) for i in range(4)])
        P.op('act', lambda e: e.activation(out=m2t[:], in_=psf(bm), func=AF.Square), reads=[('ps', bm)], writes=['m2t'], after=GA_all)
        P.op('dve', lambda e: e.tensor_tensor(out=vart[:], in0=psf(bq), in1=m2t[:], op=ALU.subtract),
             reads=[('ps', bq), 'm2t'], writes=['vart'], after=GA_all)
        P.op('dve', lambda e: e.tensor_scalar(out=vart[:], in0=vart[:], scalar1=0.0, scalar2=EPS, op0=ALU.max, op1=ALU.add),
             reads=['vart'], writes=['vart'])
        P.op('act', lambda e: e.activation(out=rstdt[:], in_=vart[:], func=AF.Sqrt), reads=['vart'], writes=['rstdt'], after=GA_all)
        P.op('dve', lambda e: e.reciprocal(out=rstdt[:], in_=rstdt[:]), reads=['rstdt'], writes=['rstdt'])
        P.op('dve', lambda e: e.scalar_tensor_tensor(out=nbt[:], in0=psf(bm), scalar=-1.0, in1=rstdt[:], op0=ALU.mult, op1=ALU.mult),
             reads=[('ps', bm), 'rstdt'], writes=['nbt'], after=GA_all)
        for i in range(4):
            lnB_chunk(b, i)

    def lnB_chunk(b, i):
        sl = i % 2
        P.op('dve', lambda e: e.tensor_tensor(out=tt[sl][:], in0=conv_blk[:, i, :], in1=rstdt[:], op=ALU.mult),
             reads=[('cb', i), 'rstdt'], writes=[('tt', sl)], after=GA_all)
        P.op('dve', lambda e: e.tensor_tensor(out=tt[sl][:], in0=tt[sl][:], in1=nbt[:], op=ALU.add),
             reads=[('tt', sl), 'nbt'], writes=[('tt', sl)])
        P.op('act', lambda e: e.activation(out=c_act[:, i, b * 512:(b + 1) * 512], in_=tt[sl][:], func=AF.Silu,
                                           bias=cw(C_LB + i), scale=cw(C_LG + i)),
             reads=[('tt', sl), 'colpack'], writes=[('ca', b, i)], after=GA_all)

    def gateC(b, j):
        blk = slice(b * 512, (b + 1) * 512)
        byc, byp, bgc, bgp = P.bank(), P.bank(), P.bank(), P.bank()

        def yc(e):
            r = None
            for i in range(4):
                r = e.matmul(psf(byc), lhsT=Wco[:, i, j * 128:(j + 1) * 128], rhs=c_act[:, i, blk], start=(i == 0), stop=(i == 3))
            return r
        P.op('pe', yc, reads=['Wco'] + [('ca', b, i_) for i_ in range(4)], writes=[('ps', byc)])
        g = j // 2
        P.op('pe', lambda e: e.matmul(psf(byp), lhsT=Wpool[:, g, (j % 2) * 128:(j % 2 + 1) * 128], rhs=dd[:, g, blk], start=True, stop=True),
             reads=['Wpool', ('d', b, g)], writes=[('ps', byp)])
        P.op('pe', mm_hT(bgc, WA, j * 128, b), reads=['WA'] + [('hT', b, jp_) for jp_ in range(4)], writes=[('ps', bgc)])
        P.op('pe', mm_hT(bgp, WA, 1024 + j * 128, b), reads=['WA'] + [('hT', b, jp_) for jp_ in range(4)], writes=[('ps', bgp)])
        sl = j % 2
        P.op('act', lambda e: e.activation(out=sgc[sl][:], in_=psf(bgc), func=AF.Sigmoid, bias=cw(C_BG + j)),
             reads=[('ps', bgc), 'colpack'], writes=[('sgc', sl)], after=GC)
        P.op('act', lambda e: e.activation(out=sgp[sl][:], in_=psf(bgp), func=AF.Sigmoid, bias=cw(C_BG + 8 + j)),
             reads=[('ps', bgp), 'colpack'], writes=[('sgp', sl)], after=GC)
        P.op('dve', lambda e: e.tensor_tensor(out=m1t[sl][:], in0=psf(byc), in1=sgc[sl][:], op=ALU.mult),
             reads=[('ps', byc), ('sgc', sl)], writes=[('m1t', sl)], after=GC)
        P.op('dve', lambda e: e.scalar_tensor_tensor(out=m2c[sl][:], in0=psf(byp), scalar=cw(C_PS + j), in1=sgp[sl][:], op0=ALU.mult, op1=ALU.mult),
             reads=[('ps', byp), ('sgp', sl), 'colpack'], writes=[('m2c', sl)], after=GC)
        mg = mgs[b % 2]
        P.op('dve', lambda e: e.tensor_tensor(out=mg[:, j, :], in0=m1t[sl][:], in1=m2c[sl][:], op=ALU.add),
             reads=[('m1t', sl), ('m2c', sl)], writes=[('mg', b % 2, j)], after=GC)

    def woC(b, slot, xs, tl, h):
        bo = P.bank()
        xt = XTC[xs]
        mg = mgs[b % 2]

        def wo(e):
            r = None
            for k in range(8):
                r = e.matmul(psf(bo), lhsT=mg[:, k, tl * 128:(tl + 1) * 128], rhs=Wout[:, k, h * 512:(h + 1) * 512],
                             start=(k == 0), stop=(k == 7))
            return r
        P.op('pe', wo, reads=[('mg', b % 2, j) for j in range(8)] + ['Wout'], writes=[('ps', bo)])
        P.op('dve', lambda e: e.tensor_tensor(out=x1t[slot][:, h * 512:(h + 1) * 512], in0=psf(bo), in1=xt[:, h * 512:(h + 1) * 512], op=ALU.add),
             reads=[('ps', bo), ('xtC', xs)], writes=[('x1t', slot, h)], after=GC)

    def loadC(s, i):
        xs = i % 4
        rows = slice(s * S + i * 128, s * S + (i + 1) * 128)
        P.op('sp', lambda e: e.dma_start(out=XTC[xs][:], in_=x[rows, :]), writes=[('xtC', xs)], dma='xtC%d' % xs, after=GC)

    def tileC(s, b, tl):
        r0 = s * S
        i = 4 * b + tl
        slot = cntC[0] % 2
        xs = i % 4
        cntC[0] += 1
        xt = XTC[xs]
        rows = slice(r0 + i * 128, r0 + (i + 1) * 128)
        if i + 3 < 16:
            loadC(s, i + 3)
        woC(b, slot, xs, tl, 0)
        woC(b, slot, xs, tl, 1)
        xk = [('x1t', slot, 0), ('x1t', slot, 1)]
        P.op('sp', lambda e: e.dma_start(out=X1[rows, :], in_=x1t[slot][:]), reads=xk, writes=[('X1', s, i)], dma='x1st%d' % slot)
        c = rms_rstd(x1t[slot][:], h2t[slot][:], xk, [('h2t', slot)])
        P.op('dve', lambda e: e.scalar_tensor_tensor(out=h2t[slot][:], in0=x1t[slot][:], scalar=st[:, c:c + 1], in1=g2b[:], op0=ALU.mult, op1=ALU.mult),
             reads=xk + [('st', c), 'g2b'], writes=[('h2t', slot)])
        P.op('sp', lambda e: e.dma_start(out=H2[rows, :], in_=h2t[slot][:]), reads=[('h2t', slot)], writes=[('H2', s, i)], dma='h2st%d' % slot)
        bt = P.bank()

        def tr2(e):
            r = None
            for k in range(8):
                r = e.transpose(out=psb(bt)[:, k * 128:(k + 1) * 128], in_=h2t[slot][:, k * 128:(k + 1) * 128], identity=ident_bf[:])
            return r
        P.op('pe', tr2, reads=[('h2t', slot), 'ident_bf'], writes=[('ps', bt)])
        P.op('act', lambda e: e.activation(out=h2T[slot][:], in_=psb(bt), func=AF.Copy), reads=[('ps', bt)], writes=[('h2T', slot)])
        bl = P.bank()

        def rt(e):
            r = None
            for k in range(8):
                r = e.matmul(psf(bl)[:, 0:NE], lhsT=h2T[slot][:, k * 128:(k + 1) * 128], rhs=wr[:, k, :], start=(k == 0), stop=(k == 7))
            return r
        P.op('pe', rt, reads=[('h2T', slot), 'wr'], writes=[('ps', bl)])
        c0, c1, c2, c3 = P.col(), P.col(), P.col(), P.col()
        P.op('dve', lambda e: e.reduce_max(out=st[:, c0:c0 + 1], in_=psf(bl)[:, 0:NE], axis=AX.X), reads=[('ps', bl)], writes=[('st', c0)])
        P.op('dve', lambda e: e.tensor_scalar(out=st[:, c1:c1 + 1], in0=st[:, c0:c0 + 1], scalar1=-1.0, scalar2=None, op0=ALU.mult),
             reads=[('st', c0)], writes=[('st', c1)])
        P.op('dve', lambda e: e.memset(st[:, c2:c2 + 1], 0.0), writes=[('st', c2)])
        es = slot
        P.op('act', lambda e: e.activation(out=ext[:, es * 16:(es + 1) * 16], in_=psf(bl)[:, 0:NE], func=AF.Exp,
                                           bias=st[:, c1:c1 + 1], accum_out=st[:, c2:c2 + 1]),
             reads=[('ps', bl), ('st', c1), ('st', c2)], writes=[('ext', es), ('st', c2)])
        P.op('dve', lambda e: e.reciprocal(out=st[:, c3:c3 + 1], in_=st[:, c2:c2 + 1]), reads=[('st', c2)], writes=[('st', c3)])
        P.op('dve', lambda e: e.tensor_scalar(out=aff_all[:, i, s * 16:(s + 1) * 16], in0=ext[:, es * 16:(es + 1) * 16],
                                              scalar1=st[:, c3:c3 + 1], scalar2=None, op0=ALU.mult),
             reads=[('ext', es), ('st', c3)], writes=[('aff', s, i)])
        if s == 0 and i == 0:
            dump("x1t0", x1t[slot][:], xk)

    stopped = False
    for s in range(NSEQ):
        load_w(WA[:, :, 0:1536], w_in[:, 0:1536], 'WA', 8)
        for i in range(4):
            zpadA(s, i)
        for b in range(4):
            for tl in range(4):
                tileA(s, b, tl)
            for jp in range(4):
                transA(s, b, jp)
            for i in range(4):
                pairA(b, i)
            for i in range(4):
                uA(b, i)
            convert_weights(2, ('u', b, 3))
            if b >= 1:
                pool_block(b - 1)
        pool_block(3)
        if s == 0:
            dump("hT", hT[:], [('hT', b, jp) for b in range(4) for jp in range(4)])
            dump("vpad", vpad[:], [('v', b, i) for b in range(4) for i in range(4)])
            dump("upad", upad[:], [('u', b, i) for b in range(4) for i in range(4)])
            dump("dd", dd[:], [('d', b, i) for b in range(4) for i in range(4)])
        if stop_after == 'A':
            P.barrier()
            stopped = True
            break
        load_w(WA, w_in[:, 1536:3584], 'WA', 8)
        if s == 1:
            zero_acc()
        for b in range(4):
            for i in range(4):
                convB(b, i)
            convert_weights(2, ('cb', 3))
            lnB(b)
        if s == 0:
            dump("c_act", c_act[:], [('ca', b, i) for b in range(4) for i in range(4)])
        if stop_after == 'B':
            P.barrier()
            stopped = True
            break
        for i in range(3):
            loadC(s, i)
        for b in range(4):
            for j in range(8):
                gateC(b, j)
            convert_weights(2, ('mg', b % 2, 7))
            if s == 0 and b == 0:
                dump("mg", mgs[0][:], [('mg', 0, j) for j in range(8)])
            for tl in range(4):
                tileC(s, b, tl)
        if s == NSEQ - 1 or stop_after == 'C':
            P.barrier()
        if stop_after == 'C':
            stopped = True
            break
    if stopped:
        return finish(nc, P, stack, dbg_outs)
    dump("aff_all", aff_all[:], [])

    em = Bump(nc, base, 8 * K, 206 * K)
    WE = [[em.take("we%d_%d" % (sl, m), [128, 8, D], BF16) for m in range(3)] for sl in range(2)]
    xg = [em.take("xg%d" % sl, [128, 4, D], BF16) for sl in range(2)]
    xgT = em.take("xgT", [128, 8, 512], BF16)
    hidT = em.take("hidT", [128, 8, 512], BF16)
    sge = [em.take("sge%d" % i, [128, 512], F32) for i in range(2)]
    ye = [em.take("ye%d" % i, [128, D], F32) for i in range(2)]
    affT = em.take("affT", [32, S], F32)
    tv = em.take("tv", [32, CAP], F32)
    ti = em.take("ti", [32, CAP], U32)
    tif = em.take("tif", [32, CAP], F32)
    idxf = em.take("idxf", [128, 2, 32], F32)
    wtop = Bump(nc, base, 186 * K, 206 * K)
    Wpg = wtop.take("Wpg", [128, 8, D], BF16)
    Wple = wtop.take("Wple", [128, 2, D], BF16)

    def load_expert(e_):
        sl = e_ % 2
        for m in range(3):
            for k2 in range(2):
                def emit(e, m=m, k2=k2):
                    return [e.dma_start(out=WE[sl][m][:, k, :], in_=W16[m][e_, k * 128:(k + 1) * 128, :]) for k in range(4 * k2, 4 * k2 + 4)]
                P.op('sp', emit, reads=[('w16', e_, m)], writes=[('we', sl, m, k) for k in range(4 * k2, 4 * k2 + 4)],
                     dma='we%d_%d_%d' % (sl, m, k2), ndma=4)

    load_expert(0)
    load_expert(1)

    def affT_blk(i4):
        bk = P.bank()

        def tra(e):
            r = None
            for q in range(4):
                r = e.matmul(psf(bk)[0:32, q * 128:(q + 1) * 128], lhsT=aff_all[:, i4 * 4 + q, :], rhs=ident_f[:], start=True, stop=True)
            return r
        P.op('pe', tra, reads=['ident_f'], writes=[('ps', bk)])
        P.op('act', lambda e: e.activation(out=affT[:, i4 * 512:(i4 + 1) * 512], in_=psf(bk)[0:32, :], func=AF.Copy),
             reads=[('ps', bk)], writes=[('affT', i4)])
    for i4 in range(4):
        affT_blk(i4)
    dump("affT", affT[:], [('affT', i4) for i4 in range(4)] + ['affTw'])

    def topk_round(r_):
        c8 = slice(r_ * 8, (r_ + 1) * 8)
        P.op('dve', lambda e: e.max(out=tv[:, c8], in_=affT[:]), reads=['affTw'] + [('affT', i4) for i4 in range(4)], writes=[('tv', r_)])
        P.op('dve', lambda e: e.max_index(out=ti[:, c8], in_max=tv[:, c8], in_values=affT[:]),
             reads=[('tv', r_), 'affTw'], writes=[('ti', r_)])
        P.op('dve', lambda e: e.match_replace(out=affT[:], in_to_replace=tv[:, c8], in_values=affT[:], imm_value=-1.0),
             reads=[('tv', r_), ('ti', r_)], writes=['affTw'])
    for r_ in range(CAP // 8):
        topk_round(r_)
    allr = [('tv', r_) for r_ in range(CAP // 8)] + [('ti', r_) for r_ in range(CAP // 8)]
    P.op('dve', lambda e: e.tensor_copy(out=tif[:], in_=ti[:]), reads=allr, writes=['tif'])
    bi, bv = P.bank(), P.bank()

    def trix(bk, src):
        def emit(e):
            r = None
            for hh in range(2):
                r = e.matmul(psf(bk)[:, hh * 32:(hh + 1) * 32], lhsT=src[:, hh * 128:(hh + 1) * 128], rhs=ident_f[0:32, 0:32], start=True, stop=True)
            return r
        return emit
    P.op('pe', trix(bi, tif), reads=['tif', 'ident_f'], writes=[('ps', bi)])
    P.op('pe', trix(bv, tv), reads=allr + ['ident_f'], writes=[('ps', bv)])
    P.op('dve', lambda e: e.tensor_copy(out=idxf[:], in_=psf(bi)[:, 0:64].rearrange("p (h c) -> p h c", h=2)), reads=[('ps', bi)], writes=['idxf'])
    P.op('dve', lambda e: e.tensor_scalar(out=idxf[:, :, 16:32], in0=idxf[:, :, 16:32], scalar1=float(S), scalar2=None, op0=ALU.add),
         reads=['idxf'], writes=['idxf'])
    P.op('dve', lambda e: e.tensor_copy(out=idxT[:], in_=idxf[:]), reads=['idxf'], writes=['idxT'])
    P.op('act', lambda e: e.activation(out=tvT[:], in_=psf(bv)[:, 0:64].rearrange("p (h c) -> p h c", h=2), func=AF.Copy),
         reads=[('ps', bv)], writes=['tvT'])
    dump("idxT", idxT[:], ['idxT'])
    dump("tvT", tvT[:], ['tvT'])
    if stop_after == 'R':
        return finish(nc, P, stack, dbg_outs)

    def gather(e_):
        sl = e_ % 2

        def emit(e):
            r = []
            for s_ in range(2):
                for hh in range(2):
                    q = s_ * 2 + hh
                    r.append(e.indirect_dma_start(out=xg[sl][:, q, :], out_offset=None, in_=H2,
                                                  in_offset=bass.IndirectOffsetOnAxis(ap=idxT[:, hh, s_ * 16 + e_: s_ * 16 + e_ + 1], axis=0)))
            return r
        P.op('pool', emit, reads=['idxT'], writes=[('xg', sl)], dma='xg%d' % sl, ndma=4)

    def trgE(sl, kp):
        bk = P.bank()

        def trg(e):
            r = None
            for kk in range(2):
                for q in range(4):
                    r = e.transpose(out=psb(bk)[:, kk * 512 + q * 128: kk * 512 + (q + 1) * 128],
                                    in_=xg[sl][:, q, (2 * kp + kk) * 128:(2 * kp + kk + 1) * 128], identity=ident_bf[:])
            return r
        P.op('pe', trg, reads=[('xg', sl), 'ident_bf'], writes=[('ps', bk)])
        src = psb(bk).rearrange("p (k c) -> p k c", k=2)
        if kp % 2 == 0:
            P.op('act', lambda e: e.activation(out=xgT[:, 2 * kp:2 * kp + 2, :], in_=src, func=AF.Copy), reads=[('ps', bk)], writes=[('xgT', kp)])
        else:
            P.op('dve', lambda e: e.tensor_copy(out=xgT[:, 2 * kp:2 * kp + 2, :], in_=src), reads=[('ps', bk)], writes=[('xgT', kp)])

    def ffn1(bk, W, f, reads):
        def emit(e):
            r = None
            for k in range(8):
                r = e.matmul(psf(bk), lhsT=W[:, k, f * 128:(f + 1) * 128], rhs=xgT[:, k, :], start=(k == 0), stop=(k == 7))
            return r
        P.op('pe', emit, reads=reads, writes=[('ps', bk)])

    def hidE(sl, f):
        Wg, Wu, Wd = WE[sl]
        xr = [('xgT', kp) for kp in range(4)]
        bg, bu = P.bank(), P.bank()
        ffn1(bg, Wg, f, xr + [('we', sl, 0, k) for k in range(8)])
        ffn1(bu, Wu, f, xr + [('we', sl, 1, k) for k in range(8)])
        s2 = f % 2
        P.op('act', lambda e: e.activation(out=sge[s2][:], in_=psf(bg), func=AF.Silu), reads=[('ps', bg)], writes=[('sge', s2)])
        P.op('dve', lambda e: e.tensor_tensor(out=hidT[:, f, :], in0=psf(bu), in1=sge[s2][:], op=ALU.mult),
             reads=[('ps', bu), ('sge', s2)], writes=[('hidT', f)])

    def downE(e_, sl, q, h):
        Wd = WE[sl][2]
        s_, hh = q // 2, q % 2
        ys = q % 2
        hr = [('hidT', f) for f in range(8)]
        bk = P.bank()

        def dn(e):
            r = None
            for f in range(8):
                r = e.matmul(psf(bk), lhsT=hidT[:, f, q * 128:(q + 1) * 128], rhs=Wd[:, f, h * 512:(h + 1) * 512], start=(f == 0), stop=(f == 7))
            return r
        P.op('pe', dn, reads=hr + [('we', sl, 2, k) for k in range(8)], writes=[('ps', bk)])
        sc_ap = tvT[:, hh, s_ * 16 + e_: s_ * 16 + e_ + 1]
        if h == 0:
            P.op('act', lambda e: e.activation(out=ye[ys][:, 0:512], in_=psf(bk), func=AF.Copy, scale=sc_ap),
                 reads=[('ps', bk), 'tvT'], writes=[('ye', ys, 0)])
        else:
            P.op('dve', lambda e: e.tensor_scalar(out=ye[ys][:, 512:1024], in0=psf(bk), scalar1=sc_ap, scalar2=None, op0=ALU.mult),
                 reads=[('ps', bk), 'tvT'], writes=[('ye', ys, 1)])

    def scatterE(e_, q):
        s_, hh = q // 2, q % 2
        ys = q % 2
        P.op('pool', lambda e: e.indirect_dma_start(
            out=ACC, out_offset=bass.IndirectOffsetOnAxis(ap=idxT[:, hh, s_ * 16 + e_: s_ * 16 + e_ + 1], axis=0),
            in_=ye[ys][:], in_offset=None, compute_op=ALU.add),
            reads=[('ye', ys, 0), ('ye', ys, 1), 'idxT'], writes=[('ACCs', s_)], dma='sc%d' % ys)

    gather(0)
    for e_ in range(NE):
        sl = e_ % 2
        if e_ + 1 < NE:
            gather(e_ + 1)
        for kp in range(4):
            trgE(sl, kp)
        for f in range(8):
            hidE(sl, f)
        for q in range(4):
            downE(e_, sl, q, 0)
            downE(e_, sl, q, 1)
            scatterE(e_, q)
        if e_ + 2 < NE:
            load_expert(e_ + 2)
        if e_ == NE - 2:
            load_w(Wpg, w_pg, 'Wpg', 8)
            load_w(Wple, w_ple, 'Wple', 2)
    P.barrier()
    if stop_after == 'E':
        return finish(nc, P, stack, dbg_outs)

    fb = Bump(nc, base, 8 * K, 186 * K)
    pool_pow[0] = True
    pgb = fb.take("pgb", [128, D], F32)
    fgb = fb.take("fgb", [128, D], F32)
    NS3 = 6
    NPF = 5
    ptf = [fb.take("ptf%d" % i, [128, 256], F32) for i in range(NS3)]
    x2 = [fb.take("x2_%d" % i, [128, D], F32) for i in range(NS3)]
    h3 = [fb.take("h3_%d" % i, [128, D], BF16) for i in range(NS3)]
    h3T = [fb.take("h3T%d" % i, [128, D], BF16) for i in range(NS3)]
    pbf = [fb.take("pbf%d" % i, [128, 256], BF16) for i in range(NS3)]
    pT = [fb.take("pT%d" % i, [128, 256], BF16) for i in range(NS3)]
    gt = [fb.take("gt%d" % i, [128, D], F32) for i in range(NS3)]
    e1 = [fb.take("e1_%d" % i, [128, D], F32) for i in range(NS3)]
    x3 = [fb.take("x3_%d" % i, [128, D], F32) for i in range(NS3)]
    ot = [fb.take("ot%d" % i, [128, D], F32) for i in range(NS3)]

    def gload(gi, gt_):
        P.op('sp', lambda e: e.dma_start(out=gt_[:], in_=gains[gi, :].partition_broadcast(128)), writes=[('gb', gi)], dma='su_gb%d' % gi)
    def fold_g3(k):
        P.op('dve', lambda e: e.tensor_scalar(out=Wpg[:, k, :], in0=Wpg[:, k, :], scalar1=cw(C_G3 + k), scalar2=None, op0=ALU.mult),
             reads=['Wpg', 'colpack'], writes=[('Wpgk', k)])
    for k in range(8):
        fold_g3(k)
    gload(3, pgb)
    gload(4, fgb)
    NTT = TOK // 128
    fstate = {}

    def f_load(i):
        slot = i % NS3
        rows = slice(i * 128, (i + 1) * 128)
        P.op('sp', lambda e: e.dma_start(out=x2[slot][:], in_=X1[rows, :]), writes=[('x2', slot)], dma='fx1_%d' % slot)
        P.op('pool', lambda e: e.dma_start(out=x2[slot][:], in_=ACC[rows, :], accum_op=ALU.add), reads=[('x2', slot)], writes=[('x2', slot)],
             dma='fac_%d' % slot)
        P.op('sp', lambda e: e.dma_start(out=ptf[slot][:], in_=p_in[rows, :]), writes=[('ptf', slot)], dma='fpt_%d' % slot)

    def f_stage1(i):
        s3, s2 = i % NS3, i % NS3
        c = rms_rstd(x2[s3][:], h3[s2][:], [('x2', s3)], [('h3', s2)])
        P.op('act', lambda e: e.activation(out=h3[s2][:], in_=x2[s3][:], func=AF.Copy, scale=st[:, c:c + 1]),
             reads=[('x2', s3), ('st', c)], writes=[('h3', s2)])
        P.op('act', lambda e: e.activation(out=pbf[s2][:], in_=ptf[s3][:], func=AF.Copy), reads=[('ptf', s3)], writes=[('pbf', s2)])

    def f_half(s2, h, ch):
        bg_, be_ = P.bank(), P.bank()

        def pg(e):
            for k in range(8):
                e.matmul(psf(bg_), lhsT=h3T[s2][:, k * 128:(k + 1) * 128], rhs=Wpg[:, k, h * 512:(h + 1) * 512], start=(k == 0), stop=False)
            return e.matmul(psf(bg_), lhsT=ones_row[0:1, :], rhs=bple_row[0:1, h * 512:(h + 1) * 512], start=False, stop=True)
        P.op('pe', pg, reads=[('h3T', s2), 'Wpg', 'ones_row', 'bple_row'] + [('Wpgk', k) for k in range(8)], writes=[('ps', bg_)])

        def pe_(e):
            r = None
            for k in range(2):
                r = e.matmul(psf(be_), lhsT=pT[s2][:, k * 128:(k + 1) * 128], rhs=Wple[:, k, h * 512:(h + 1) * 512], start=(k == 0), stop=(k == 1))
            return r
        P.op('pe', pe_, reads=[('pT', s2), 'Wple'], writes=[('ps', be_)])
        P.op('act', lambda e: e.activation(out=gt[s2][:, h * 512:(h + 1) * 512], in_=psf(bg_), func=AF.Sigmoid), reads=[('ps', bg_)], writes=[('gt', s2, h)])
        P.op('act', lambda e: e.activation(out=e1[s2][:, h * 512:(h + 1) * 512], in_=psf(be_), func=AF.Copy), reads=[('ps', be_)], writes=[('e1', s2, h)])
        return be_

    def f_stage2(i):
        s2 = i % NS3
        bt, bp = P.bank(), P.bank()

        def tr3(e):
            r = None
            for k in range(8):
                r = e.transpose(out=psb(bt)[:, k * 128:(k + 1) * 128], in_=h3[s2][:, k * 128:(k + 1) * 128], identity=ident_bf[:])
            return r
        P.op('pe', tr3, reads=[('h3', s2), 'ident_bf'], writes=[('ps', bt)])

        def tr4(e):
            r = None
            for k in range(2):
                r = e.transpose(out=psb(bp)[:, k * 128:(k + 1) * 128], in_=pbf[s2][:, k * 128:(k + 1) * 128], identity=ident_bf[:])
            return r
        P.op('pe', tr4, reads=[('pbf', s2), 'ident_bf'], writes=[('ps', bp)])
        P.op('dve', lambda e: e.tensor_copy(out=h3T[s2][:], in_=psb(bt)), reads=[('ps', bt)], writes=[('h3T', s2)])
        P.op('dve', lambda e: e.tensor_copy(out=pT[s2][:], in_=psb(bp)[:, 0:256]), reads=[('ps', bp)], writes=[('pT', s2)])
        f_half(s2, 0, None)
        f_half(s2, 1, None)

    def f_e1(s2, h, cr):
        hs = slice(h * 512, (h + 1) * 512)
        P.op('dve', lambda e: e.scalar_tensor_tensor(out=e1[s2][:, hs], in0=e1[s2][:, hs], scalar=st[:, cr:cr + 1], in1=pgb[:, hs], op0=ALU.mult, op1=ALU.mult),
             reads=[('e1', s2, h), ('st', cr), ('gb', 3)], writes=[('e1', s2, h)])

    def f_stage3(i):
        s3, s2 = i % NS3, i % NS3
        cr = rms_rstd(e1[s2][:], x3[s2][:], [('e1', s2, 0), ('e1', s2, 1)], [('x3', s2)])
        f_e1(s2, 0, cr)
        f_e1(s2, 1, cr)
        P.op('dve', lambda e: e.tensor_tensor(out=x3[s2][:], in0=gt[s2][:], in1=e1[s2][:], op=ALU.mult),
             reads=[('gt', s2, 0), ('gt', s2, 1), ('e1', s2, 0), ('e1', s2, 1)], writes=[('x3', s2)])
        P.op('dve', lambda e: e.tensor_tensor(out=x3[s2][:], in0=x3[s2][:], in1=x2[s3][:], op=ALU.add), reads=[('x3', s2), ('x2', s3)], writes=[('x3', s2)])
        c2_ = rms_rstd(x3[s2][:], ot[s2][:], [('x3', s2)], [('ot', s2)])
        P.op('dve', lambda e: e.scalar_tensor_tensor(out=ot[s2][:], in0=x3[s2][:], scalar=st[:, c2_:c2_ + 1], in1=fgb[:], op0=ALU.mult, op1=ALU.mult),
             reads=[('x3', s2), ('st', c2_), ('gb', 4)], writes=[('ot', s2)])
        P.op('sp', lambda e: e.dma_start(out=out[i * 128:(i + 1) * 128, :], in_=ot[s2][:]), reads=[('ot', s2)], writes=[('out', i)], dma='ost%d' % s2)

    for i in range(min(NPF, NTT)):
        f_load(i)
    for i in range(NTT):
        if i + NPF < NTT:
            f_load(i + NPF)
        f_stage1(i)
        f_stage2(i)
        f_stage3(i)
    return finish(nc, P, stack, dbg_outs)


def finish(nc, P, stack, dbg_outs):
    streams = P.finalize()
    print("[sched] ops=%d est_us=%.1f" % (len(P.ops), P.est_us))
    with stack:
        with nc.Block() as block:
            @block.tensor
            def _(e):
                for f in streams['pe']:
                    f(e)

            @block.scalar
            def _(e):
                for f in streams['act']:
                    f(e)

            @block.vector
            def _(e):
                for f in streams['dve']:
                    f(e)

            @block.gpsimd
            def _(e):
                for f in streams['pool']:
                    f(e)

            @block.sync
            def _(e):
                for f in streams['sp']:
                    f(e)
    return nc, dbg_outs


def make_in_maps(inputs, ncores=8):
    g = lambda k: np.asarray(inputs[k], dtype=np.float32)
    x = g("x")
    p = g("p")[0]
    conv_w = g("conv_w")[0]
    colpack = np.zeros((128, 168), np.float32)
    colpack[:, 0:16] = g("b_gate")[0].reshape(16, 128).T
    colpack[:, 16:140] = conv_w.T.reshape(4, 128, 31).transpose(1, 0, 2).reshape(128, 124)
    colpack[:, 140:144] = g("conv_b")[0].reshape(4, 128).T
    colpack[:, 144:148] = g("conv_ln_g")[0].reshape(4, 128).T
    colpack[:, 148:152] = g("conv_ln_b")[0].reshape(4, 128).T
    colpack[:, 152:160] = g("pool_scale")[0].reshape(8, 128).T
    colpack[:, 160:168] = g("norm3_g")[0].reshape(8, 128).T
    gains = np.stack([g("norm1_g")[0], g("norm2_g")[0], g("norm3_g")[0], g("ple_norm_g")[0], g("final_g")], axis=0)
    shared = {
        "w_in": np.ascontiguousarray(g("w_in")[0]),
        "w_conv_out": np.ascontiguousarray(g("w_conv_out")[0]),
        "w_pool": np.ascontiguousarray(g("w_pool")[0]),
        "w_out": np.ascontiguousarray(g("w_out")[0]),
        "w_router": np.ascontiguousarray(g("w_router")[0]),
        "w_exp_gate": np.ascontiguousarray(g("w_exp_gate")[0]),
        "w_exp_up": np.ascontiguousarray(g("w_exp_up")[0]),
        "w_exp_down": np.ascontiguousarray(g("w_exp_down")[0]),
        "w_ple_gate": np.ascontiguousarray(g("w_ple_gate")[0]),
        "w_ple": np.ascontiguousarray(g("w_ple")[0]),
        "b_ple_gate": np.ascontiguousarray(g("b_ple_gate")[0].reshape(1, D)),
        "gains": np.ascontiguousarray(gains),
        "colpack": colpack,
        "ident": np.eye(128, dtype=np.float32),
        "zeros": np.zeros((512, D), np.float32),
    }
    maps = []
    for c in range(ncores):
        m = dict(shared)
        m["x"] = np.ascontiguousarray(x[2 * c:2 * c + 2].reshape(TOK, D))
        m["p"] = np.ascontiguousarray(p[2 * c:2 * c + 2].reshape(TOK, 256))
        maps.append(m)
    return maps


_CACHE = {}


def kernel(**inputs):
    if "nc" not in _CACHE:
        _CACHE["nc"] = build()[0]
    nc = _CACHE["nc"]
    maps = make_in_maps(inputs)
    res = run_bass_kernel_spmd(nc, maps, core_ids=list(range(8)))
    outs = [np.asarray(r["out"], dtype=np.float32).reshape(2, S, D) for r in res.results]
    return np.concatenate(outs, axis=0)
```
